# Optimizing a Trainium2 kernel written in Bass

```python
import math
import jax, jax.numpy as jnp
from jax import lax
import numpy as np

D_MODEL = 1024
BATCH = 2
SEQ = 8192
DEPTH = 2
DEC_BATCH = 2
DEC_SEQ = 16384
PAST_LEN = 128

HEAD_DIM = 64
A_HEADS = 16
A_KV_HEADS = 4
A_GROUP = A_HEADS // A_KV_HEADS
Q_BLOCK = 128
GRID_W = 64
ROPE_THETA = 10000.0
ROPE_AXIS_DIM = HEAD_DIM // 2
B_HEADS = 16
DILATED_CONFIGS = ((128, 1), (512, 4), (2048, 16))
B_GROUPS = len(DILATED_CONFIGS)
NUM_BUCKETS = 32
MAX_DISTANCE = 1024
N_EXPERTS = 32
TOP_K = 4
D_FF_EXPERT = D_MODEL
SWIGLU_ALPHA = 1.702
SWIGLU_LIMIT = 7.0
EXPERT_BLOCK = 128
RMS_EPS = 1e-6
NEG_INF = -1e30

kernel_name = 'hybrid_dilated_gqa_moe_encoder'


def _rms(x, g):
    xf = x.astype(jnp.float32)
    y = xf * lax.rsqrt(jnp.mean(xf * xf, axis=-1, keepdims=True) + RMS_EPS)
    return (y * g.astype(jnp.float32)).astype(x.dtype)


def _axial_rope_tables(seq_len):
    rows = seq_len // GRID_W
    row = jnp.repeat(jnp.arange(rows, dtype=jnp.float32), GRID_W)
    col = jnp.tile(jnp.arange(GRID_W, dtype=jnp.float32), rows)
    n_freq = ROPE_AXIS_DIM // 2
    inv_freq = 1.0 / (ROPE_THETA ** (jnp.arange(n_freq, dtype=jnp.float32) / n_freq))
    ang_r = row[:, None] * inv_freq[None, :]
    ang_c = col[:, None] * inv_freq[None, :]
    ang = jnp.concatenate([ang_r, ang_r, ang_c, ang_c], axis=-1)
    return jnp.cos(ang), jnp.sin(ang)


def _apply_axial_rope(x, cos, sin):
    xf = x.astype(jnp.float32)
    r1, r2, c1, c2 = jnp.split(xf, 4, axis=-1)
    rot = jnp.concatenate([-r2, r1, -c2, c1], axis=-1)
    return (xf * cos[None, :, None, :] + rot * sin[None, :, None, :]).astype(x.dtype)


def _t5_bucket(rel):
    nb = NUM_BUCKETS // 2
    max_exact = nb // 2
    ret = (rel > 0).astype(np.int32) * nb
    n = np.abs(rel)
    large = max_exact + (np.log(np.maximum(n, 1) / max_exact) / np.log(MAX_DISTANCE / max_exact)
                         * (nb - max_exact)).astype(np.int32)
    large = np.minimum(large, nb - 1)
    return (ret + np.where(n < max_exact, n, large)).astype(np.int32)


def _mixer_a(h, w_qkv, gq, gk, w_o):
    Bb, S, _ = h.shape
    qkv = h @ w_qkv
    nq_cols = A_HEADS * HEAD_DIM
    nk_cols = A_KV_HEADS * HEAD_DIM
    q = qkv[..., :nq_cols].reshape(Bb, S, A_HEADS, HEAD_DIM)
    k = qkv[..., nq_cols:nq_cols + nk_cols].reshape(Bb, S, A_KV_HEADS, HEAD_DIM)
    v = qkv[..., nq_cols + nk_cols:].reshape(Bb, S, A_KV_HEADS, HEAD_DIM)
    cos, sin = _axial_rope_tables(S)
    q = _apply_axial_rope(_rms(q, gq), cos, sin)
    k = _apply_axial_rope(_rms(k, gk), cos, sin)
    nq = S // Q_BLOCK
    qb = q.reshape(Bb, nq, Q_BLOCK, A_KV_HEADS, A_GROUP, HEAD_DIM).transpose(1, 0, 2, 3, 4, 5)
    scale = HEAD_DIM ** -0.5

    def block(qi):
        s = jnp.einsum('bqhge,bkhe->bhgqk', qi, k).astype(jnp.float32) * scale
        p = jax.nn.softmax(s, axis=-1)
        return jnp.einsum('bhgqk,bkhe->bqhge', p.astype(v.dtype), v)

    o = lax.map(block, qb)
    o = o.transpose(1, 0, 2, 3, 4, 5).reshape(Bb, S, A_HEADS * HEAD_DIM)
    return o @ w_o


def _dilated_group(q, k, v, bias_tab, window, dilation):
    Bb, S, H, hd = q.shape
    radius = window // (2 * dilation)
    blk = radius
    L = S // dilation
    nb = -(-L // blk)
    Lp = nb * blk

    def sub(t):
        return t.reshape(Bb, L, dilation, H, hd).transpose(0, 2, 1, 3, 4)

    qs, ks, vs = sub(q), sub(k), sub(v)
    qs = jnp.pad(qs, ((0, 0), (0, 0), (0, Lp - L), (0, 0), (0, 0))).reshape(Bb, dilation, nb, blk, H, hd)

    def windows(t):
        tp = jnp.pad(t, ((0, 0), (0, 0), (blk, Lp - L + blk), (0, 0), (0, 0)))
        tp = tp.reshape(Bb, dilation, nb + 2, blk, H, hd)
        return jnp.concatenate([tp[:, :, :-2], tp[:, :, 1:-1], tp[:, :, 2:]], axis=3)

    kw, vw = windows(ks), windows(vs)
    qq = np.arange(blk)[:, None]
    kk = np.arange(3 * blk)[None, :]
    rel = kk - blk - qq
    bucket = _t5_bucket(rel * dilation)
    key_pos = np.arange(nb)[:, None] * blk - blk + np.arange(3 * blk)[None, :]
    mask = (np.abs(rel) <= radius)[None] & ((key_pos >= 0) & (key_pos < L))[:, None, :]
    bias = bias_tab[jnp.asarray(bucket)].transpose(2, 0, 1).astype(jnp.float32)
    s = jnp.einsum('bdnqhe,bdnkhe->bdnhqk', qs, kw).astype(jnp.float32) * (hd ** -0.5) + bias
    s = jnp.where(jnp.asarray(mask)[:, None], s, NEG_INF)
    lse = jax.nn.logsumexp(s, axis=-1)
    p = jnp.exp(s - lse[..., None])
    o = jnp.einsum('bdnhqk,bdnkhe->bdnqhe', p.astype(v.dtype), vw)
    o = o.reshape(Bb, dilation, Lp, H, hd)[:, :, :L].transpose(0, 2, 1, 3, 4).reshape(Bb, S, H, hd)
    lse = lse.transpose(0, 1, 2, 4, 3).reshape(Bb, dilation, Lp, H)[:, :, :L]
    lse = lse.transpose(0, 2, 1, 3).reshape(Bb, S, H)
    return o, lse


def _mixer_b(h, w_qkv, gq, gk, w_o, rel_bias):
    Bb, S, _ = h.shape
    qkv = (h @ w_qkv).reshape(Bb, S, B_GROUPS, 3, B_HEADS, HEAD_DIM)
    outs, lses = [], []
    for g, (window, dilation) in enumerate(DILATED_CONFIGS):
        q = _rms(qkv[:, :, g, 0], gq[g])
        k = _rms(qkv[:, :, g, 1], gk[g])
        v = qkv[:, :, g, 2]
        o, lse = _dilated_group(q, k, v, rel_bias[:, g * B_HEADS:(g + 1) * B_HEADS], window, dilation)
        outs.append(o)
        lses.append(lse)
    wts = jax.nn.softmax(jnp.stack(lses), axis=0)
    o = jnp.sum(wts[..., None] * jnp.stack(outs).astype(jnp.float32), axis=0).astype(h.dtype)
    return o.reshape(Bb, S, B_HEADS * HEAD_DIM) @ w_o


def _moe(h, w_router, b_router, w_gu, b_gu, w_dn, b_dn):
    Bb, S, D = h.shape
    T = Bb * S
    xt = h.reshape(T, D)
    logits = (xt @ w_router + b_router).astype(jnp.float32)
    top_val, top_idx = lax.top_k(logits, TOP_K)
    gate = jax.nn.softmax(top_val, axis=-1).astype(h.dtype)
    TK = T * TOP_K
    flat_e = top_idx.reshape(TK)
    flat_tok = jnp.arange(TK, dtype=jnp.int32) // TOP_K
    order = jnp.argsort(flat_e)
    e_sorted = flat_e[order]
    counts = jnp.bincount(flat_e, length=N_EXPERTS)
    padded = (counts + EXPERT_BLOCK - 1) // EXPERT_BLOCK * EXPERT_BLOCK
    start = jnp.cumsum(counts) - counts
    pend = jnp.cumsum(padded)
    pstart = pend - padded
    dest = pstart[e_sorted] + jnp.arange(TK, dtype=jnp.int32) - start[e_sorted]
    n_rows = (TK + N_EXPERTS * (EXPERT_BLOCK - 1) + EXPERT_BLOCK - 1) // EXPERT_BLOCK * EXPERT_BLOCK
    n_blk = n_rows // EXPERT_BLOCK
    row_tok = jnp.full((n_rows,), T, jnp.int32).at[dest].set(flat_tok[order])
    row_gate = jnp.zeros((n_rows,), h.dtype).at[dest].set(gate.reshape(TK)[order])
    blk_start = jnp.arange(n_blk, dtype=jnp.int32) * EXPERT_BLOCK
    blk_e = jnp.minimum(jnp.sum(blk_start[:, None] >= pend[None, :], axis=1), N_EXPERTS - 1)
    x_pad = jnp.concatenate([xt, jnp.zeros((1, D), xt.dtype)], axis=0)
    xs = x_pad[row_tok].reshape(n_blk, EXPERT_BLOCK, D)

    def expert_block(args):
        xb, e = args
        gu = xb @ w_gu[e] + b_gu[e]
        g = jnp.minimum(gu[:, :D_FF_EXPERT], SWIGLU_LIMIT)
        u = jnp.clip(gu[:, D_FF_EXPERT:], -SWIGLU_LIMIT, SWIGLU_LIMIT)
        act = g * jax.nn.sigmoid(SWIGLU_ALPHA * g)
        return ((u + 1.0) * act) @ w_dn[e] + b_dn[e]

    ys = lax.map(expert_block, (xs, blk_e)).reshape(n_rows, D)
    out = jnp.zeros((T + 1, D), h.dtype).at[row_tok].add(ys * row_gate[:, None])[:T]
    return out.reshape(Bb, S, D)


def _trunk(x, c, norm_mix_g, norm_ffn_g, w_ada, b_ada, w_qkv_a, q_norm_a, k_norm_a, w_o_a,
           w_qkv_b, q_norm_b, k_norm_b, w_o_b, rel_bias, w_router, b_router,
           w_gate_up, b_gate_up, w_down, b_down):
    for i in range(DEPTH):
        mod = (jax.nn.silu(c) @ w_ada[i] + b_ada[i])[:, None, :]
        shift_m, scale_m, gate_m, shift_f, scale_f, gate_f = jnp.split(mod, 6, axis=-1)
        hm = _rms(x, norm_mix_g[i]) * (1.0 + scale_m) + shift_m
        j = i // 2
        if i % 2 == 0:
            mix = _mixer_a(hm, w_qkv_a[j], q_norm_a[j], k_norm_a[j], w_o_a[j])
        else:
            mix = _mixer_b(hm, w_qkv_b[j], q_norm_b[j], k_norm_b[j], w_o_b[j], rel_bias)
        x = x + gate_m * mix
        hf = _rms(x, norm_ffn_g[i]) * (1.0 + scale_f) + shift_f
        x = x + gate_f * _moe(hf, w_router[i], b_router[i], w_gate_up[i], b_gate_up[i],
                              w_down[i], b_down[i])
    return x


def setup_inputs(seed: int = 0) -> dict:
    key = jax.random.key(seed)
    ks = jax.random.split(key, 24)
    n_a = (DEPTH + 1) // 2
    n_b = DEPTH // 2
    D = D_MODEL
    f32 = jnp.float32
    qkv_a = (A_HEADS + 2 * A_KV_HEADS) * HEAD_DIM
    qkv_b = B_GROUPS * 3 * B_HEADS * HEAD_DIM
    nrm = lambda k, s: jax.random.normal(k, s, f32)
    return {
        'x_prompt': nrm(ks[0], (BATCH, SEQ, D)),
        'x_sample': nrm(ks[1], (DEC_BATCH, DEC_SEQ, D)),
        'c_prompt': nrm(ks[2], (BATCH, D)),
        'c_sample': nrm(ks[3], (DEC_BATCH, D)),
        'norm_mix_g': 1.0 + 0.02 * nrm(ks[4], (DEPTH, D)),
        'norm_ffn_g': 1.0 + 0.02 * nrm(ks[5], (DEPTH, D)),
        'w_ada': nrm(ks[6], (DEPTH, D, 6 * D)) * (0.5 * D ** -0.5),
        'b_ada': 0.02 * nrm(ks[7], (DEPTH, 6 * D)),
        'w_qkv_a': nrm(ks[8], (n_a, D, qkv_a)) * D ** -0.5,
        'q_norm_a': 1.0 + 0.02 * nrm(ks[9], (n_a, HEAD_DIM)),
        'k_norm_a': 1.0 + 0.02 * nrm(ks[10], (n_a, HEAD_DIM)),
        'w_o_a': nrm(ks[11], (n_a, A_HEADS * HEAD_DIM, D)) * (A_HEADS * HEAD_DIM) ** -0.5,
        'w_qkv_b': nrm(ks[12], (n_b, D, qkv_b)) * D ** -0.5,
        'q_norm_b': 1.0 + 0.02 * nrm(ks[13], (n_b, B_GROUPS, HEAD_DIM)),
        'k_norm_b': 1.0 + 0.02 * nrm(ks[14], (n_b, B_GROUPS, HEAD_DIM)),
        'w_o_b': nrm(ks[15], (n_b, B_HEADS * HEAD_DIM, D)) * (B_HEADS * HEAD_DIM) ** -0.5,
        'rel_bias': 0.5 * nrm(ks[16], (NUM_BUCKETS, B_GROUPS * B_HEADS)),
        'w_router': nrm(ks[17], (DEPTH, D, N_EXPERTS)) * D ** -0.5,
        'b_router': 0.01 * nrm(ks[18], (DEPTH, N_EXPERTS)),
        'w_gate_up': nrm(ks[19], (DEPTH, N_EXPERTS, D, 2 * D_FF_EXPERT)) * D ** -0.5,
        'b_gate_up': 0.01 * nrm(ks[20], (DEPTH, N_EXPERTS, 2 * D_FF_EXPERT)),
        'w_down': nrm(ks[21], (DEPTH, N_EXPERTS, D_FF_EXPERT, D)) * D_FF_EXPERT ** -0.5,
        'b_down': 0.01 * nrm(ks[22], (DEPTH, N_EXPERTS, D)),
    }


def reference(x_prompt, x_sample, c_prompt, c_sample, norm_mix_g, norm_ffn_g, w_ada, b_ada,
              w_qkv_a, q_norm_a, k_norm_a, w_o_a, w_qkv_b, q_norm_b, k_norm_b, w_o_b, rel_bias,
              w_router, b_router, w_gate_up, b_gate_up, w_down, b_down):
    y_prompt = _trunk(x_prompt, c_prompt, norm_mix_g, norm_ffn_g, w_ada, b_ada, w_qkv_a, q_norm_a,
                      k_norm_a, w_o_a, w_qkv_b, q_norm_b, k_norm_b, w_o_b, rel_bias, w_router,
                      b_router, w_gate_up, b_gate_up, w_down, b_down)
    y_sample = _trunk(x_sample, c_sample, norm_mix_g, norm_ffn_g, w_ada, b_ada, w_qkv_a, q_norm_a,
                      k_norm_a, w_o_a, w_qkv_b, q_norm_b, k_norm_b, w_o_b, rel_bias, w_router,
                      b_router, w_gate_up, b_gate_up, w_down, b_down)
    return (y_prompt, y_sample)
```

```python
from contextlib import ExitStack
import numpy as np
import concourse.bass as bass
import concourse.mybir as mybir
from concourse.bass_utils import run_bass_kernel_spmd

F32 = mybir.dt.float32
BF16 = mybir.dt.bfloat16
I32 = mybir.dt.int32
ALU = mybir.AluOpType
AF = mybir.ActivationFunctionType
AX = mybir.AxisListType

D = 1024
KC = 8
HD = 64
H = 1024
NEG = -30000.0
RMS_EPS = 1e-6
DIL = (1, 4, 16)
TOPK = 4
BS = 512

DEBUG = {}


class Cfg:
    def __init__(self, S0=16384, S1=8192, NE=32, FF=1024, stop_after=None):
        self.S = (S0, S1)
        self.T = (S0 // 4, S1 // 4)
        self.E = (self.T[0] + 2 * H, self.T[1] + 2 * H)
        self.NE = NE
        self.FF = FF
        self.stop_after = stop_after
        assert self.T[0] % 2048 == 0 and self.T[1] % 2048 == 0
        self.ET = self.E[0] + self.E[1]
        self.TT = self.T[0] + self.T[1]

    def nblk(self, ntok):
        return (ntok * TOPK + self.NE * (BS - 1) + BS - 1) // BS


class Buf:
    __slots__ = ("name", "w", "r", "dsem")

    def __init__(self, name):
        self.name = name
        self.w = []
        self.r = []
        self.dsem = None


class Op:
    __slots__ = ("eng", "fn", "deps", "dma", "dsem", "dval", "signal", "sig", "ident")

    def __init__(self, eng, fn):
        self.eng = eng
        self.fn = fn
        self.deps = []
        self.dma = False
        self.dsem = None
        self.dval = 0
        self.signal = False
        self.sig = 0


ENGS = ("sp", "act", "pool", "dve", "pe")
N_DSEM = 56


class Sched:
    def __init__(self):
        self.ops = {e: [] for e in ENGS}
        self.dsem_count = [0] * N_DSEM
        self.dsem_next = 0
        self.phase_bufs = []
        self.dma_since_barrier = {}

    def buf(self, name):
        b = Buf(name)
        self.phase_bufs.append(b)
        return b

    def _dsem_for(self, b):
        if b.dsem is None:
            assert self.dsem_next < N_DSEM, "out of dma sems in this phase"
            b.dsem = self.dsem_next
            self.dsem_next += 1
        return b.dsem

    @staticmethod
    def _merge(lst, ev):
        if ev[0] == "dma":
            for i, o in enumerate(lst):
                if o[0] == "dma" and o[1] == ev[1]:
                    if o[2] < ev[2]:
                        lst[i] = ev
                    return
        else:
            for i, o in enumerate(lst):
                if o[0] == "op" and o[1].eng == ev[1].eng:
                    if o[1].ident < ev[1].ident:
                        lst[i] = ev
                    return
        lst.append(ev)

    def op(self, eng, fn, reads=(), writes=(), dma_buf=None):
        o = Op(eng, fn)
        o.ident = len(self.ops[eng])
        deps = []
        for b in reads:
            for ev in b.w:
                self._merge(deps, ev)
        for b in writes:
            for ev in b.w:
                self._merge(deps, ev)
            for ev in b.r:
                self._merge(deps, ev)
        if dma_buf is not None:
            o.dma = True
            o.dsem = self._dsem_for(dma_buf)
            self.dsem_count[o.dsem] += 16
            o.dval = self.dsem_count[o.dsem]
            ev = ("dma", o.dsem, o.dval)
            self.dma_since_barrier[o.dsem] = o.dval
        else:
            ev = ("op", o)
        for d in deps:
            if d[0] == "op":
                if d[1].eng == "pe" and eng == "pe":
                    continue
                d[1].signal = True
            o.deps.append(d)
        for b in writes:
            b.w = [ev]
            b.r = []
        for b in reads:
            if b in writes:
                continue
            self._merge(b.r, ev)
        self.ops[eng].append(o)
        return o

    def barrier(self):
        o = Op("sp", lambda e: e.nop())
        o.ident = len(self.ops["sp"])
        for e in ENGS:
            if e == "sp":
                continue
            for cand in reversed(self.ops[e]):
                if not cand.dma and cand.fn is not None:
                    cand.signal = True
                    o.deps.append(("op", cand))
                    break
        for s_, v in self.dma_since_barrier.items():
            o.deps.append(("dma", s_, v))
        self.dma_since_barrier = {}
        o.signal = True
        self.ops["sp"].append(o)
        for e in ENGS:
            if e == "sp":
                continue
            w = Op(e, None)
            w.ident = len(self.ops[e])
            w.deps.append(("op", o))
            self.ops[e].append(w)
        for b in self.phase_bufs:
            b.w = []
            b.r = []
            b.dsem = None
        self.phase_bufs = [b for b in self.phase_bufs if b.name.startswith("dram:")]
        self.dsem_next = 0
        return o

    def emit(self, nc, stack):
        esem = {e: stack.enter_context(nc.semaphore("es_" + e)) for e in ENGS}
        dsem = [stack.enter_context(nc.semaphore("ds%d" % i)) for i in range(N_DSEM)]
        for e in ENGS:
            c = 0
            for o in self.ops[e]:
                if o.signal and not o.dma:
                    c += 1
                    o.sig = c
        if DEBUG.get("verbose"):
            print("signals", {e: max([o.sig for o in self.ops[e]] + [0]) for e in ENGS}, "dma sem max", max(self.dsem_count),
                  "ops", {e: len(self.ops[e]) for e in ENGS})
        block = stack.enter_context(nc.Block())
        handles = {"sp": block.sync, "act": block.scalar, "pool": block.gpsimd,
                   "dve": block.vector, "pe": block.tensor}

        def make(e):
            def body(eng):
                waited = {}
                for o in self.ops[e]:
                    for d in o.deps:
                        if d[0] == "op":
                            key, val = ("e", d[1].eng), d[1].sig
                            sem = esem[d[1].eng]
                            assert val > 0, (e, d[1].eng, d[1].ident, d[1].fn, o.ident)
                        else:
                            key, val = ("d", d[1]), d[2]
                            sem = dsem[d[1]]
                        if waited.get(key, 0) >= val:
                            continue
                        waited[key] = val
                        eng.wait_ge(sem, val)
                    if o.fn is None:
                        continue
                    ins = o.fn(eng)
                    if o.dma:
                        ins.then_inc(dsem[o.dsem], 16)
                    elif o.signal:
                        ins.then_inc(esem[e], 1)
            return body

        for e in ENGS:
            handles[e](make(e))


class Builder:
    def __init__(self, cfg):
        self.cfg = cfg
        self.nc = bass.Bass("TRN2", target_bir_lowering=False)
        self.s = Sched()
        self.stack = ExitStack()
        self.dram = {}
        self.dbuf = {}
        self.arena_off = 0
        self.ARENA = 196 * 1024

    def din(self, name, shape, dt=F32):
        if name in DEBUG.get("skip_inputs", ()):
            t = self.nc.dram_tensor(name, list(shape), dt, kind="Internal")
            self.dram[name] = t
            return t
        t = self.nc.dram_tensor(name, list(shape), dt, kind="ExternalInput")
        self.dram[name] = t
        return t

    def dout(self, name, shape, dt=F32):
        t = self.nc.dram_tensor(name, list(shape), dt, kind="ExternalOutput")
        self.dram[name] = t
        self.dbuf[name] = self.s.buf("dram:" + name)
        return t

    def dscr(self, name, shape, dt=F32):
        kind = "ExternalOutput" if name in DEBUG.get("dump", ()) else "Internal"
        t = self.nc.dram_tensor(name, list(shape), dt, kind=kind)
        self.dram[name] = t
        self.dbuf[name] = self.s.buf("dram:" + name)
        return t

    def reset_arena(self, keep=0):
        self.arena_off = keep

    def sb(self, name, shape, dt, parts=128):
        esz = 2 if dt == BF16 else 4
        n = int(np.prod(shape))
        nbytes = (n * esz + 31) // 32 * 32
        off = self.arena_off
        self.arena_off += nbytes
        assert self.arena_off <= self.ARENA, (name, self.arena_off)
        ap = self.arena[:, off // 2: off // 2 + (n * esz) // 2]
        if esz == 4:
            ap = ap.bitcast(dt)
        if len(shape) == 2:
            ap = ap.rearrange("p (a b) -> p a b", a=shape[0])
        elif len(shape) == 3:
            ap = ap.rearrange("p (a b c) -> p a b c", a=shape[0], b=shape[1])
        elif len(shape) == 4:
            ap = ap.rearrange("p (a b c d) -> p a b c d", a=shape[0], b=shape[1], c=shape[2])
        return ap, self.s.buf(name)

    def pool(self, name, shape, dt, n):
        return [self.sb("%s%d" % (name, i), shape, dt) for i in range(n)]


def build(cfg):
    B = Builder(cfg)
    nc, s, st = B.nc, B.s, B.stack
    NE, FF = cfg.NE, cfg.FF
    S, T, E = cfg.S, cfg.T, cfg.E
    SP0 = S[0] + 2 * H

    xs = [B.din("xs0", [S[0], D]), B.din("xs1", [S[1], D])]
    xw = B.din("xw", [cfg.ET, D])
    cvec = B.din("cvec", [2, D])
    norm_mix_g = B.din("norm_mix_g", [2, D]); norm_ffn_g = B.din("norm_ffn_g", [2, D])
    w_ada = B.din("w_ada", [2, D, 6 * D]); b_ada = B.din("b_ada", [2, 6 * D])
    w_qkv_a = B.din("w_qkv_a", [D, 1536]); q_norm_a = B.din("q_norm_a", [1, 64]); k_norm_a = B.din("k_norm_a", [1, 64])
    w_o_a = B.din("w_o_a", [D, D])
    w_qkv_b = B.din("w_qkv_b", [D, 9216]); q_norm_b = B.din("q_norm_b", [3, 64]); k_norm_b = B.din("k_norm_b", [3, 64])
    w_o_b = B.din("w_o_b", [D, D]); rel_bias = B.din("rel_bias", [32, 48])
    w_router = B.din("w_router", [2, D, NE]); b_router = B.din("b_router", [2, NE])
    w_gu = B.din("w_gate_up", [2 * NE, D, 2 * FF]); b_gu = B.din("b_gate_up", [2 * NE * (2 * FF // 128), 128])
    w_dn = B.din("w_down", [2 * NE, FF, D]); b_dn = B.din("b_down", [2 * NE, D])
    c_ident = B.din("c_ident", [128, 128]); c_anti = B.din("c_anti", [128, 128])
    c_ustrict = B.din("c_ustrict", [128, 128])
    c_une = B.din("c_une", [2, NE, NE])
    NBMAX = cfg.nblk(cfg.ET)
    c_iotab = B.din("c_iotab", [NE, NBMAX])
    ropek = B.din("ropek", [S[0], 128])
    ropew = B.din("ropew", [cfg.ET, 128])
    c_piota = B.din("c_piota", [128, 1])
    NKM = sum(DIL[g] * (T[sg] // (128 * DIL[g]) + 1) for g in range(3) for sg in range(2))
    kmaskT = B.din("kmaskT", [128, NKM])
    c_oh = B.din("c_oh", [3, 2, 33, 256])

    yout = B.dout("y", [cfg.TT, D])

    modv = B.dscr("modv", [2, 2, 6, D])
    kT0 = [B.dscr("kT0_%d" % i, [128, 2, S[i]], BF16) for i in range(2)]
    vx0 = [B.dscr("vx0_%d" % i, [S[i], 4, 65], BF16) for i in range(2)]
    qT0 = [B.dscr("qT0_%d" % i, [128, 2, 4, E[i]], BF16) for i in range(2)]
    x1 = B.dscr("x1", [cfg.ET, D])
    x2 = B.dscr("x2", [cfg.ET, D])
    x3 = B.dscr("x3", [cfg.TT, D])
    hfb = B.dscr("hfb", [cfg.ET, D], BF16)
    NR0 = NBMAX * BS
    xsr = B.dscr("xsr", [NR0, D], BF16)
    ysr = B.dscr("ysr", [NR0, D])
    NFC = FF // 128
    wgb = B.dscr("wgb", [2 * NE * 128, 8 * 2 * FF], BF16)
    wdb = B.dscr("wdb", [2 * NE * 128, NFC * D], BF16)
    fvs = B.dscr("fvs", [3, 2, 16, 256])
    NI = [[T[sg] // DIL[g] + 128 for sg in range(2)] for g in range(3)]
    kT1 = [[B.dscr("kT1_%d_%d" % (g, sg), [128, 8, DIL[g] * NI[g][sg]], BF16) for sg in range(2)] for g in range(3)]
    qT1 = [[B.dscr("qT1_%d_%d" % (g, sg), [128, 8, DIL[g] * NI[g][sg]], BF16) for sg in range(2)] for g in range(3)]
    vx1 = [[B.dscr("vx1_%d_%d" % (g, sg), [DIL[g] * NI[g][sg], 16, 65], BF16) for sg in range(2)] for g in range(3)]
    num1 = [B.dscr("num1_%d" % g, [cfg.TT, 16, 65]) for g in range(3)]

    def db(name):
        return B.dbuf[name]

    _bc = {}

    def bchk(e, n):
        if n not in _bc:
            r = e.alloc_register("bc%d" % len(_bc))
            e.reg_mov(r, n)
            _bc[n] = e.snap(r, min_val=n, max_val=n)
        return _bc[n]

    B.arena = st.enter_context(nc.sbuf_tensor("arena", [128, B.ARENA // 2], BF16))
    psum = st.enter_context(nc.psum_tensor("ps", [128, 8, 512], F32))
    PS = [(psum[:, i, :], s.buf("ps%d" % i)) for i in range(8)]

    def psb(i):
        return psum[:, i, :].bitcast(BF16)

    id32, id32B = B.sb("id32", [128], F32)
    idb, idbB = B.sb("idb", [128], BF16)
    antib, antibB = B.sb("antib", [128], BF16)
    ones_b, onesB = B.sb("ones_b", [512], BF16)
    ones_f, onesfB = B.sb("ones_f", [128], F32)
    tmpc, tmpcB = B.sb("tmpc", [128], F32)
    s.op("sp", lambda e: e.dma_start(out=id32, in_=c_ident.ap()), writes=[id32B], dma_buf=id32B)
    s.op("sp", lambda e: e.dma_start(out=tmpc, in_=c_anti.ap()), writes=[tmpcB], dma_buf=tmpcB)
    s.op("dve", lambda e: e.tensor_copy(out=idb, in_=id32), reads=[id32B], writes=[idbB])
    s.op("dve", lambda e: e.tensor_copy(out=antib, in_=tmpc), reads=[tmpcB], writes=[antibB])
    s.op("dve", lambda e: e.memset(ones_b, 1.0), writes=[onesB])
    s.op("dve", lambda e: e.memset(ones_f, 1.0), writes=[onesfB])
    KEEP = B.arena_off

    def load_bc(eng_name, dst, dstB, src_row_ap, nparts=128):
        s.op(eng_name, lambda e: e.dma_start(out=dst, in_=src_row_ap.partition_broadcast(nparts)),
             writes=[dstB], dma_buf=dstB)

    def rstd(ss, ssB, n_inv):
        s.op("act", lambda e: e.activation(out=ss, in_=ss, func=AF.Ln, scale=n_inv, bias=epsc[:, 0:1]),
             reads=[ssB, epscB], writes=[ssB])
        s.op("act", lambda e: e.activation(out=ss, in_=ss, func=AF.Exp, scale=-0.5), reads=[ssB], writes=[ssB])

    def norm_tile(x, xB, Abc, Bbc, bcB, sq, sqB, ss, ssB, t32, t32B, out, outB):
        s.op("act", lambda e: e.activation(out=sq, in_=x, func=AF.Square), reads=[xB], writes=[sqB])
        s.op("dve", lambda e: e.reduce_sum(out=ss, in_=sq, axis=AX.X), reads=[sqB], writes=[ssB])
        rstd(ss, ssB, 1.0 / D)
        s.op("dve", lambda e: e.scalar_tensor_tensor(out=t32, in0=x, scalar=ss[:, 0:1], in1=Abc, op0=ALU.mult, op1=ALU.mult),
             reads=[xB, ssB, bcB], writes=[t32B])
        s.op("dve", lambda e: e.tensor_tensor(out=out, in0=t32, in1=Bbc, op=ALU.add), reads=[t32B, bcB], writes=[outB])

    def transpose8(src, srcB, ident, identB, bank, dst, dstB, eng="act"):
        pv = psb(bank)

        def f(e):
            for k in range(8):
                ins = e.transpose(out=pv[:, k * 128:(k + 1) * 128], in_=src[:, k * 128:(k + 1) * 128], identity=ident)
            return ins
        s.op("pe", f, reads=[srcB, identB], writes=[PS[bank][1]])
        if eng == "act":
            s.op("act", lambda e: e.copy(out=dst.rearrange("p a b -> p (a b)"), in_=pv), reads=[PS[bank][1]], writes=[dstB])
        else:
            s.op("dve", lambda e: e.tensor_copy(out=dst.rearrange("p a b -> p (a b)"), in_=pv), reads=[PS[bank][1]], writes=[dstB])

    def proj(hmT, hmTB, W, WB, c0, ncols, bank):
        def f(e):
            for k in range(8):
                ins = e.matmul(PS[bank][0][:, 0:ncols], lhsT=hmT[:, k, :], rhs=W[:, k, c0:c0 + ncols], start=(k == 0), stop=(k == 7))
            return ins
        s.op("pe", f, reads=[hmTB, WB], writes=[PS[bank][1]])

    def headnorm(src, srcB, nh, gbc, gbcB, sq, sqB, ssh, sshB, out, outB):
        s.op("act", lambda e: e.activation(out=sq[:, 0:nh * 64], in_=src, func=AF.Square), reads=[srcB], writes=[sqB])
        s.op("dve", lambda e: e.reduce_sum(out=ssh[:, 0:nh], in_=sq[:, 0:nh * 64].rearrange("p (h c) -> p h c", c=64), axis=AX.X),
             reads=[sqB], writes=[sshB])
        s.op("act", lambda e: e.activation(out=ssh[:, 0:nh], in_=ssh[:, 0:nh], func=AF.Ln, scale=1.0 / 64, bias=epsc[:, 0:1]),
             reads=[sshB, epscB], writes=[sshB])
        s.op("act", lambda e: e.activation(out=ssh[:, 0:nh], in_=ssh[:, 0:nh], func=AF.Exp, scale=-0.5), reads=[sshB], writes=[sshB])
        s3 = src.rearrange("p (h c) -> p h c", c=64)
        o3 = out.rearrange("p (h c) -> p h c", c=64)
        s.op("dve", lambda e: e.tensor_tensor(out=o3, in0=s3, in1=ssh[:, 0:nh].unsqueeze(2).to_broadcast([128, nh, 64]), op=ALU.mult),
             reads=[srcB, sshB], writes=[outB])
        s.op("dve", lambda e: e.tensor_tensor(out=o3, in0=o3, in1=gbc.unsqueeze(1).to_broadcast([128, nh, 64]), op=ALU.mult),
             reads=[outB, gbcB], writes=[outB])

    def rope(src, srcB, nh, cs, csB, sw, swB, t1, t1B, out, outB):
        s5 = src.rearrange("p (h a w c) -> p h a w c", a=2, w=2, c=16)
        w5 = sw[:, 0:nh * 64].rearrange("p (h a w c) -> p h a w c", a=2, w=2, c=16)
        s.op("act", lambda e: e.copy(out=w5[:, :, :, 0, :], in_=s5[:, :, :, 1, :]), reads=[srcB], writes=[swB])
        s.op("act", lambda e: e.copy(out=w5[:, :, :, 1, :], in_=s5[:, :, :, 0, :]), reads=[srcB, swB], writes=[swB])
        cosb = cs[:, 0:64].unsqueeze(1).to_broadcast([128, nh, 64])
        sinb = cs[:, 64:128].unsqueeze(1).to_broadcast([128, nh, 64])
        s3 = src.rearrange("p (h c) -> p h c", c=64)
        w3 = sw[:, 0:nh * 64].rearrange("p (h c) -> p h c", c=64)
        a3 = t1[:, 0:nh * 64].rearrange("p (h c) -> p h c", c=64)
        o3 = out.rearrange("p (h c) -> p h c", c=64)
        s.op("dve", lambda e: e.tensor_tensor(out=a3, in0=s3, in1=cosb, op=ALU.mult), reads=[srcB, csB], writes=[t1B])
        s.op("dve", lambda e: e.tensor_tensor(out=w3, in0=w3, in1=sinb, op=ALU.mult), reads=[swB, csB], writes=[swB])
        s.op("dve", lambda e: e.tensor_tensor(out=o3, in0=a3, in1=w3, op=ALU.add), reads=[t1B, swB], writes=[outB])

    def load_w_bf16(dst, dstB, src2d, c0, ncols):
        s.op("pool", lambda e: e.dma_start(out=dst, in_=src2d[:, c0:c0 + ncols].rearrange("(k p) c -> p k c", p=128)),
             writes=[dstB], dma_buf=dstB)

    B.reset_arena(KEEP)
    epsc, epscB = B.sb("epsc", [1], F32)
    s.op("dve", lambda e: e.memset(epsc, RMS_EPS), writes=[epscB])
    KEEP = B.arena_off
    cT, cTB = B.sb("cT", [8, 2], F32)
    mrow, mrowB = B.sb("mrow", [6 * D], F32)
    brow, browB = B.sb("brow", [6 * D], F32)
    grow, growB = B.sb("grow", [2, D], F32)
    wa = B.pool("wa", [8, 512], F32, 2)
    for g_ in range(2):
        def f_ct(e, g_=g_):
            with nc.allow_non_contiguous_dma(reason="tiny transposed load of c"):
                return e.dma_start(out=cT[:, :, g_], in_=cvec.ap()[g_].rearrange("(k p) -> p k", p=128))
        s.op("sp", f_ct, writes=[cTB], dma_buf=cTB)
    s.op("act", lambda e: e.activation(out=cT, in_=cT, func=AF.Silu), reads=[cTB], writes=[cTB])
    for l in range(2):
        load_bc("sp", brow[0:2, :], browB, b_ada.ap()[l:l + 1, :], 2)
        load_bc("sp", grow[0:2, 0, :], growB, norm_mix_g.ap()[l:l + 1, :], 2)
        load_bc("sp", grow[0:2, 1, :], growB, norm_ffn_g.ap()[l:l + 1, :], 2)
        for cc in range(12):
            wt, wtB = wa[cc % 2]
            s.op("sp", lambda e, wt=wt, cc=cc, l=l: e.dma_start(out=wt, in_=w_ada.ap()[l, :, cc * 512:(cc + 1) * 512].rearrange("(k p) c -> p k c", p=128)),
                 writes=[wtB], dma_buf=wtB)
            bank = cc % 2

            def f(e, wt=wt, bank=bank):
                for k in range(8):
                    ins = e.matmul(PS[bank][0][0:2, :], lhsT=cT[:, k, :], rhs=wt[:, k, :], start=(k == 0), stop=(k == 7))
                return ins
            s.op("pe", f, reads=[cTB, wtB], writes=[PS[bank][1]])
            s.op("dve", lambda e, bank=bank, cc=cc: e.tensor_tensor(out=mrow[0:2, cc * 512:(cc + 1) * 512], in0=PS[bank][0][0:2, :],
                                                                  in1=brow[0:2, cc * 512:(cc + 1) * 512], op=ALU.add),
                 reads=[PS[bank][1], browB], writes=[mrowB])
        for j, gi in ((1, 0), (4, 1)):
            s.op("dve", lambda e, j=j, gi=gi: e.scalar_tensor_tensor(out=mrow[0:2, j * D:(j + 1) * D], in0=mrow[0:2, j * D:(j + 1) * D], scalar=1.0,
                                                                  in1=grow[0:2, gi, :], op0=ALU.add, op1=ALU.mult),
                 reads=[mrowB, growB], writes=[mrowB])
        s.op("sp", lambda e, l=l: e.dma_start(out=modv.ap()[l].rearrange("g s d -> g (s d)"), in_=mrow[0:2, :]),
             reads=[mrowB], writes=[db("modv")], dma_buf=mrowB)
    s.barrier()
    if cfg.stop_after == "P0":
        return finish(B, cfg)

    B.reset_arena(KEEP)
    Wa, WaB = B.sb("Wa", [8, 1536], BF16)
    load_w_bf16(Wa, WaB, w_qkv_a.ap(), 0, 1536)
    gq, gqB = B.sb("gq", [64], F32); gk, gkB = B.sb("gk", [64], F32)
    load_bc("sp", gq, gqB, q_norm_a.ap()[0:1, :]); load_bc("sp", gk, gkB, k_norm_a.ap()[0:1, :])
    bcm = [B.sb("bcm%d" % i, [2, D], F32) for i in range(2)]
    for sg in range(2):
        load_bc("sp", bcm[sg][0][:, 0, :], bcm[sg][1], modv.ap()[0, sg, 1:2, :].rearrange("a d -> a d"))
        load_bc("sp", bcm[sg][0][:, 1, :], bcm[sg][1], modv.ap()[0, sg, 0:1, :])
    xt = B.pool("xt", [D], F32, 2)
    cst = B.pool("cst", [128], F32, 2)
    sq, sqB = B.sb("sq", [D], F32)
    ss, ssB = B.sb("ss", [1], F32)
    t32, t32B = B.sb("t32", [D], F32)
    hm = B.pool("hm", [D], BF16, 2)
    hmT = B.pool("hmT", [8, 128], BF16, 2)
    qf, qfB = B.sb("qf", [D], F32)
    qn, qnB = B.sb("qn", [D], F32)
    sw, swB = B.sb("sw", [D], F32)
    t1, t1B = B.sb("t1", [D], F32)
    ssh, sshB = B.sb("ssh", [16], F32)
    kb = B.pool("kb", [256], BF16, 2)
    qb = B.pool("qb", [D], BF16, 2)
    vxt = B.pool("vxt", [4, 65], BF16, 2)
    for i in range(2):
        s.op("dve", lambda e, i=i: e.memset(vxt[i][0], 1.0), writes=[vxt[i][1]])
    kTt = B.pool("kTt", [2, 128], BF16, 2)
    qTt = B.pool("qTt", [8, 128], BF16, 2)
    qbn, qbnB = B.sb("qbn", [D], BF16)
    segoff = (0, E[0])
    it = 0
    for sg in range(2):
        ntk = S[sg] // 128
        nte = E[sg] // 128
        for j in range(ntk + nte):
            isq = j >= ntk
            jj = j - ntk if isq else j
            x, xB = xt[it % 2]; cs, csB = cst[it % 2]
            hmb, hmbB = hm[it % 2]; hT, hTB = hmT[it % 2]
            if not isq:
                r0 = jj * 128
                s.op("sp", lambda e, x=x, r0=r0, sg=sg: e.dma_start(out=x, in_=xs[sg].ap()[r0:r0 + 128, :]), writes=[xB], dma_buf=xB)
                s.op("sp", lambda e, cs=cs, r0=r0: e.dma_start(out=cs, in_=ropek.ap()[r0:r0 + 128, :]), writes=[csB], dma_buf=csB)
            else:
                r0 = segoff[sg] + jj * 128
                s.op("sp", lambda e, x=x, r0=r0: e.dma_start(out=x, in_=xw.ap()[r0:r0 + 128, :]), writes=[xB], dma_buf=xB)
                s.op("sp", lambda e, cs=cs, r0=r0: e.dma_start(out=cs, in_=ropew.ap()[r0:r0 + 128, :]), writes=[csB], dma_buf=csB)
            A_, Bc_ = bcm[sg][0][:, 0, :], bcm[sg][0][:, 1, :]
            norm_tile(x, xB, A_, Bc_, bcm[sg][1], sq, sqB, ss, ssB, t32, t32B, hmb, hmbB)
            transpose8(hmb, hmbB, idb, idbB, 0 + (it % 2), hT, hTB)
            if not isq:
                proj(hT, hTB, Wa, WaB, 1024, 512, 2 + (it % 2))
                pk = PS[2 + (it % 2)]
                s.op("act", lambda e, pk=pk: e.copy(out=qf[:, 0:512], in_=pk[0]), reads=[pk[1]], writes=[qfB])
                headnorm(qf[:, 0:256], qfB, 4, gk, gkB, sq, sqB, ssh, sshB, qn[:, 0:256], qnB)
                kbt, kbB = kb[it % 2]
                rope(qn[:, 0:256], qnB, 4, cs, csB, sw, swB, t1, t1B, kbt, kbB)
                vt, vtB = vxt[it % 2]
                s.op("dve", lambda e, vt=vt: e.tensor_copy(out=vt[:, :, 0:64], in_=qf[:, 256:512].rearrange("p (h c) -> p h c", c=64)),
                     reads=[qfB], writes=[vtB])
                s.op("sp", lambda e, vt=vt, jj=jj, sg=sg: e.dma_start(out=vx0[sg].ap()[jj * 128:(jj + 1) * 128, :, :], in_=vt),
                     reads=[vtB], writes=[db("vx0_%d" % sg)], dma_buf=vtB)
                bank = 4 + (it % 2)
                pv = psb(bank)

                def ft(e, kbt=kbt, pv=pv):
                    for p in range(2):
                        ins = e.transpose(out=pv[:, p * 128:(p + 1) * 128], in_=kbt[:, p * 128:(p + 1) * 128], identity=idb)
                    return ins
                s.op("pe", ft, reads=[kbB, idbB], writes=[PS[bank][1]])
                kt, ktB = kTt[it % 2]
                s.op("act", lambda e, kt=kt, pv=pv: e.copy(out=kt.rearrange("p a b -> p (a b)"), in_=pv[:, 0:256]), reads=[PS[bank][1]], writes=[ktB])
                s.op("sp", lambda e, kt=kt, jj=jj, sg=sg: e.dma_start(out=kT0[sg].ap()[:, :, jj * 128:(jj + 1) * 128], in_=kt),
                     reads=[ktB], writes=[db("kT0_%d" % sg)], dma_buf=ktB)
            else:
                proj(hT, hTB, Wa, WaB, 0, 512, 2)
                proj(hT, hTB, Wa, WaB, 512, 512, 3)
                s.op("act", lambda e: e.copy(out=qf[:, 0:512], in_=PS[2][0]), reads=[PS[2][1]], writes=[qfB])
                s.op("act", lambda e: e.copy(out=qf[:, 512:1024], in_=PS[3][0]), reads=[PS[3][1], qfB], writes=[qfB])
                headnorm(qf, qfB, 16, gq, gqB, sq, sqB, ssh, sshB, qn, qnB)
                qbt, qbB = qb[it % 2]
                rope(qn, qnB, 16, cs, csB, sw, swB, t1, t1B, qbn, qbnB)
                srcv = qbn.rearrange("q (p u i c) -> q p u i c", p=2, u=2, i=4)
                dstv = qbt.rearrange("q (p i u c) -> q p i u c", p=2, i=4, u=2)
                for u_ in range(2):
                    s.op("act", lambda e, u_=u_, srcv=srcv, dstv=dstv: e.copy(out=dstv[:, :, :, u_, :], in_=srcv[:, :, u_, :, :]),
                         reads=[qbnB, qbB], writes=[qbB])
                bank = 4 + (it % 2)
                pv = psb(bank)

                def ftq(e, qbt=qbt, pv=pv):
                    for pi in range(8):
                        ins = e.transpose(out=pv[:, pi * 128:(pi + 1) * 128], in_=qbt[:, pi * 128:(pi + 1) * 128], identity=idb)
                    return ins
                s.op("pe", ftq, reads=[qbB, idbB], writes=[PS[bank][1]])
                qt, qtB = qTt[it % 2]
                s.op("act", lambda e, qt=qt, pv=pv: e.copy(out=qt.rearrange("p a b -> p (a b)"), in_=pv), reads=[PS[bank][1]], writes=[qtB])
                s.op("sp", lambda e, qt=qt, jj=jj, sg=sg: e.dma_start(
                    out=qT0[sg].ap().rearrange("p a i n -> p (a i) n")[:, :, jj * 128:(jj + 1) * 128], in_=qt),
                    reads=[qtB], writes=[db("qT0_%d" % sg)], dma_buf=qtB)
            it += 1
    s.barrier()
    if cfg.stop_after == "P1":
        return finish(B, cfg)

    oT0 = [B.dscr("oT0_%d" % i, [64, 16, E[i]], BF16) for i in range(2)]

    def attn_seg(sg):
        B.reset_arena(KEEP)
        Sn = S[sg]
        nkt = Sn // 128
        KT, KTB = B.sb("KT", [Sn], BF16)
        VX, VXB = B.sb("VX", [nkt, 2, 65], BF16)
        QTp = B.pool("QTp", [4, 128], BF16, 2)
        pTp = B.pool("pTp", [2, 512], BF16, 3)
        o32, o32B = B.sb("o32", [512], F32)
        rc, rcB = B.sb("rc", [512], F32)
        oTn = B.pool("oTn", [512], BF16, 2)
        cnt = 0
        for p in range(2):
            s.op("sp", lambda e, p=p, sg=sg: e.dma_start(out=KT, in_=kT0[sg].ap()[:, p, :]), reads=[db("kT0_%d" % sg)], writes=[KTB], dma_buf=KTB)
            for t0 in range(0, nkt, 16):
                s.op("sp", lambda e, p=p, sg=sg, t0=t0: e.dma_start(
                    out=VX[:, t0:t0 + 16, :, :], in_=vx0[sg].ap()[t0 * 128:(t0 + 16) * 128, 2 * p:2 * p + 2, :].rearrange("(t k) u c -> k t u c", k=128)),
                    reads=[db("vx0_%d" % sg)], writes=[VXB], dma_buf=VXB)
            for jq in range(E[sg] // 128):
                QT, QTB = QTp[jq % 2]
                s.op("sp", lambda e, QT=QT, p=p, jq=jq, sg=sg: e.dma_start(out=QT, in_=qT0[sg].ap()[:, p, :, jq * 128:(jq + 1) * 128]),
                     reads=[db("qT0_%d" % sg)], writes=[QTB], dma_buf=QTB)
                for u in range(2):
                    acc = 4 + (cnt % 2)
                    cnt += 1
                    lo, hi = 64 * u, 64 * u + 64
                    nst = nkt // 2
                    for st_ in range(nst):
                        sb0 = 2 * (st_ % 2)

                        def fs(e, st_=st_, sb0=sb0, lo=lo, hi=hi, QT=QT):
                            for t_ in range(2):
                                k0 = (2 * st_ + t_) * 128
                                ins = e.matmul(PS[sb0 + t_][0], lhsT=KT[lo:hi, k0:k0 + 128],
                                               rhs=QT[lo:hi, :, :].rearrange("p a b -> p (a b)"), start=True, stop=True)
                            return ins
                        s.op("pe", fs, reads=[KTB, QTB], writes=[PS[sb0][1], PS[sb0 + 1][1]])
                        pT, pTB = pTp[st_ % 3]
                        s.op("act", lambda e, sb0=sb0, pT=pT: e.activation(out=pT.rearrange("p a b -> p (a b)"),
                                                                        in_=psum[:, sb0:sb0 + 2, :].rearrange("p a b -> p (a b)"),
                                                                        func=AF.Exp, scale=0.125),
                             reads=[PS[sb0][1], PS[sb0 + 1][1]], writes=[pTB])

                        def fv(e, st_=st_, pT=pT, u=u, acc=acc, nst=nst):
                            for t_ in range(2):
                                kt_ = 2 * st_ + t_
                                ins = e.matmul(PS[acc][0][0:65, :], lhsT=VX[:, kt_, u, :], rhs=pT[:, t_, :],
                                               start=(kt_ == 0), stop=(kt_ == 2 * nst - 1))
                            return ins
                        s.op("pe", fv, reads=[VXB, pTB], writes=[PS[acc][1]])
                    s.op("act", lambda e, acc=acc: e.copy(out=o32[0:65, :], in_=PS[acc][0][0:65, :]), reads=[PS[acc][1]], writes=[o32B])
                    s.op("pe", lambda e: e.matmul(PS[6][0][0:64, :], lhsT=ones_f[64:65, 0:64], rhs=o32[64:65, :], start=True, stop=True),
                         reads=[o32B, onesfB], writes=[PS[6][1]])
                    s.op("dve", lambda e: e.reciprocal(out=rc[0:64, :], in_=PS[6][0][0:64, :]), reads=[PS[6][1]], writes=[rcB])
                    on, onB = oTn[cnt % 2]
                    s.op("dve", lambda e, on=on: e.tensor_tensor(out=on[0:64, :], in0=o32[0:64, :], in1=rc[0:64, :], op=ALU.mult),
                         reads=[o32B, rcB], writes=[onB])
                    h = 2 * p + u
                    s.op("sp", lambda e, on=on, h=h, jq=jq, sg=sg: e.dma_start(
                        out=oT0[sg].ap()[:, 4 * h:4 * h + 4, jq * 128:(jq + 1) * 128], in_=on[0:64, :].rearrange("p (a b) -> p a b", a=4)),
                        reads=[onB], writes=[db("oT0_%d" % sg)], dma_buf=onB)
        s.barrier()
    for sg_i in range(2):
        attn_seg(sg_i)
    if cfg.stop_after == "P2b1":
        return finish(B, cfg)

    def wo_phase(l, oT_fn, oTbufs, x_loader, gate_grp, ntiles_seg, dst, dstname, w_o_t):
        B.reset_arena(KEEP)
        Wo, WoB = B.sb("Wo", [16, D], BF16)
        s.op("pool", lambda e: e.dma_start(out=Wo[0:64], in_=w_o_t.ap().rearrange("(h c) n -> c h n", c=64)), writes=[WoB], dma_buf=WoB)
        gbc = [B.sb("gbc%d" % i, [D], F32) for i in range(2)]
        for g_ in range(2):
            load_bc("sp", gbc[g_][0], gbc[g_][1], modv.ap()[l, g_, 2:3, :])
        oTt = B.pool("oTt", [16, 128], BF16, 2)
        xp = B.pool("xp", [D], F32, 2)
        tmp = B.pool("tmpo", [D], F32, 2)
        row = 0
        it_ = 0
        for sg in range(2):
            for jq in range(ntiles_seg[sg]):
                oTl, oTlB = oTt[it_ % 2]
                s.op("sp", lambda e, oTl=oTl, sg=sg, jq=jq: e.dma_start(out=oTl[0:64], in_=oT_fn(sg, jq)), reads=oTbufs, writes=[oTlB], dma_buf=oTlB)
                x, xB = xp[it_ % 2]
                x_loader(x, xB, sg, jq)
                tm, tmB = tmp[it_ % 2]
                for hf in range(2):
                    bank = 2 * (it_ % 2) + hf

                    def fw(e, oTl=oTl, hf=hf, bank=bank):
                        for hd in range(16):
                            ins = e.matmul(PS[bank][0], lhsT=oTl[0:64, hd, :], rhs=Wo[0:64, hd, hf * 512:(hf + 1) * 512], start=(hd == 0), stop=(hd == 15))
                        return ins
                    s.op("pe", fw, reads=[oTlB, WoB], writes=[PS[bank][1]])
                    s.op("dve", lambda e, tm=tm, hf=hf, bank=bank, sg=sg: e.tensor_tensor(out=tm[:, hf * 512:(hf + 1) * 512], in0=PS[bank][0],
                                                                                 in1=gbc[gate_grp(sg)][0][:, hf * 512:(hf + 1) * 512], op=ALU.mult),
                         reads=[PS[bank][1], gbc[gate_grp(sg)][1]], writes=[tmB])
                s.op("dve", lambda e, tm=tm, x=x: e.tensor_tensor(out=tm, in0=tm, in1=x, op=ALU.add), reads=[tmB, xB], writes=[tmB])
                s.op("sp", lambda e, tm=tm, row=row: e.dma_start(out=dst.ap()[row:row + 128, :], in_=tm), reads=[tmB], writes=[db(dstname)], dma_buf=tmB)
                row += 128
                it_ += 1
        s.barrier()

    def x0_loader(x, xB, sg, jq):
        r0 = segoff[sg] + jq * 128
        s.op("sp", lambda e, x=x, r0=r0: e.dma_start(out=x, in_=xw.ap()[r0:r0 + 128, :]), writes=[xB], dma_buf=xB)

    wo_phase(0, lambda sg, jq: oT0[sg].ap()[:, :, jq * 128:(jq + 1) * 128], [db("oT0_0"), db("oT0_1")], x0_loader,
             lambda sg: sg, [E[0] // 128, E[1] // 128], x1, "x1", w_o_a)
    if cfg.stop_after == "P2":
        return finish(B, cfg)

    def moe(l, src, srcname, ntiles, grp_of_tile, dst, dstname):
        NT = ntiles
        NB = cfg.nblk(NT * 128)
        NR = NB * BS
        B.reset_arena(KEEP)
        G4, G4B = B.sb("G4", [NT, 4], F32)
        D4i, D4iB = B.sb("D4i", [NT, 4], I32)
        idxE, idxEB = B.sb("idxE", [NB], I32)
        idxW, idxWB = B.sb("idxW", [NB], I32)
        idx16, idx16B = B.sb("idx16", [NB], I32)
        KEEP2 = B.arena_off
        L_all, L_allB = B.sb("L_all", [NT, NE], F32)
        M_all, M_allB = B.sb("M_all", [NT, NE], F32)
        M4, M4B = B.sb("M4", [NT, 8], F32)
        D4, D4B = B.sb("D4", [NT, 4], F32)
        KEEP3 = B.arena_off
        Wr, WrB = B.sb("Wr", [8, NE], F32)
        s.op("sp", lambda e: e.dma_start(out=Wr, in_=w_router.ap()[l].rearrange("(k p) n -> p k n", p=128)), writes=[WrB], dma_buf=WrB)
        brb, brbB = B.sb("brb", [NE], F32)
        load_bc("sp", brb, brbB, b_router.ap()[l:l + 1, :])
        bcf = [B.sb("bcf%d" % i, [2, D], F32) for i in range(2)]
        for g_ in range(2):
            load_bc("sp", bcf[g_][0][:, 0, :], bcf[g_][1], modv.ap()[l, g_, 4:5, :])
            load_bc("sp", bcf[g_][0][:, 1, :], bcf[g_][1], modv.ap()[l, g_, 3:4, :])
        xp = B.pool("xm", [D], F32, 2)
        sq, sqB = B.sb("sqm", [D], F32)
        ss, ssB = B.sb("ssm", [1], F32)
        t32, t32B = B.sb("t32m", [D], F32)
        hf32 = B.pool("hf32", [D], F32, 2)
        hfbf = B.pool("hfbf", [D], BF16, 2)
        hfT = B.pool("hfT", [8, 128], F32, 2)
        e4, e4B = B.sb("e4", [4], F32)
        s4, s4B = B.sb("s4", [1], F32)
        for j in range(NT):
            x, xB = xp[j % 2]
            s.op("sp", lambda e, x=x, j=j: e.dma_start(out=x, in_=src.ap()[j * 128:(j + 1) * 128, :]), reads=[db(srcname)], writes=[xB], dma_buf=xB)
            g_ = grp_of_tile(j)
            h32, h32B = hf32[j % 2]
            norm_tile(x, xB, bcf[g_][0][:, 0, :], bcf[g_][0][:, 1, :], bcf[g_][1], sq, sqB, ss, ssB, t32, t32B, h32, h32B)
            hb, hbB = hfbf[j % 2]
            s.op("act", lambda e, hb=hb, h32=h32: e.copy(out=hb, in_=h32), reads=[h32B], writes=[hbB])
            s.op("sp", lambda e, hb=hb, j=j: e.dma_start(out=hfb.ap()[j * 128:(j + 1) * 128, :], in_=hb), reads=[hbB], writes=[db("hfb")], dma_buf=hbB)
            hT, hTB = hfT[j % 2]
            b0 = 2 * (j % 2)

            def ftr(e, h32=h32, b0=b0):
                for k in range(8):
                    ins = e.transpose(out=PS[b0 + k // 4][0][:, (k % 4) * 128:(k % 4 + 1) * 128], in_=h32[:, k * 128:(k + 1) * 128], identity=id32)
                return ins
            s.op("pe", ftr, reads=[h32B, id32B], writes=[PS[b0][1], PS[b0 + 1][1]])
            s.op("act", lambda e, hT=hT, b0=b0: e.copy(out=hT.rearrange("p a b -> p (a b)"), in_=psum[:, b0:b0 + 2, :].rearrange("p a b -> p (a b)")),
                 reads=[PS[b0][1], PS[b0 + 1][1]], writes=[hTB])
            lb = 4 + (j % 2)

            def frt(e, hT=hT, lb=lb):
                for k in range(8):
                    ins = e.matmul(PS[lb][0][:, 0:NE], lhsT=hT[:, k, :], rhs=Wr[:, k, :], start=(k == 0), stop=(k == 7))
                return ins
            s.op("pe", frt, reads=[hTB, WrB], writes=[PS[lb][1]])
            s.op("dve", lambda e, j=j, lb=lb: e.tensor_tensor(out=L_all[:, j, :], in0=PS[lb][0][:, 0:NE], in1=brb, op=ALU.add),
                 reads=[PS[lb][1], brbB], writes=[L_allB])
            s.op("dve", lambda e, j=j: e.max(out=M4[:, j, :], in_=L_all[:, j, :]), reads=[L_allB], writes=[M4B])
            s.op("dve", lambda e, j=j: e.tensor_scalar(out=M_all[:, j, :], in0=L_all[:, j, :], scalar1=M4[:, j, 3:4], scalar2=None, op0=ALU.is_ge),
                 reads=[L_allB, M4B], writes=[M_allB])
            s.op("dve", lambda e, j=j: e.tensor_scalar(out=e4, in0=M4[:, j, 0:4], scalar1=M4[:, j, 0:1], scalar2=None, op0=ALU.subtract),
                 reads=[M4B], writes=[e4B])
            s.op("act", lambda e: e.activation(out=e4, in_=e4, func=AF.Exp), reads=[e4B], writes=[e4B])
            s.op("dve", lambda e: e.reduce_sum(out=s4, in_=e4, axis=AX.X), reads=[e4B], writes=[s4B])
            s.op("dve", lambda e: e.reciprocal(out=s4, in_=s4), reads=[s4B], writes=[s4B])
            s.op("dve", lambda e, j=j: e.tensor_scalar(out=G4[:, j, :], in0=e4, scalar1=s4[:, 0:1], scalar2=None, op0=ALU.mult),
                 reads=[e4B, s4B], writes=[G4B])
        s.barrier()
        B.reset_arena(KEEP3)
        ustr, ustrB = B.sb("ustr", [128], F32)
        s.op("sp", lambda e: e.dma_start(out=ustr, in_=c_ustrict.ap()), writes=[ustrB], dma_buf=ustrB)
        une, uneB = B.sb("une", [2, NE], F32)
        s.op("sp", lambda e: e.dma_start(out=une[0:NE], in_=c_une.ap().rearrange("a e f -> e a f")), writes=[uneB], dma_buf=uneB)
        iot, iotB = B.sb("iot", [NB], F32)
        s.op("sp", lambda e: e.dma_start(out=iot[0:NE], in_=c_iotab.ap()[:, 0:NB]), writes=[iotB], dma_buf=iotB)
        cn, cnB = B.sb("cn", [4], F32)
        pbc, pbcB = B.sb("pbc", [128], F32)
        cmp_, cmpB = B.sb("cmp", [NB], F32)
        bke, bkeB = B.sb("bke", [NB], F32)
        bki, bkiB = B.sb("bki", [NB], F32)
        pio, pioB = B.sb("pio", [1], F32)
        s.op("sp", lambda e: e.dma_start(out=pio, in_=c_piota.ap()), writes=[pioB], dma_buf=pioB)
        PBR, PBRB = B.sb("PBR", [NE], F32)
        dj, djB = B.sb("dj", [NE], F32)
        tq, tqB = B.sb("tq", [NE], F32)

        def fcnt(e):
            for j in range(NT):
                ins = e.matmul(PS[0][0][0:NE, 0:1], lhsT=M_all[:, j, :], rhs=ones_f[:, 0:1], start=(j == 0), stop=(j == NT - 1))
            return ins
        s.op("pe", fcnt, reads=[M_allB, onesfB], writes=[PS[0][1]])
        s.op("dve", lambda e: e.tensor_copy(out=cn[0:NE, 0:1], in_=PS[0][0][0:NE, 0:1]), reads=[PS[0][1]], writes=[cnB])
        s.op("dve", lambda e: e.tensor_scalar(out=cmp_[0:NE, :], in0=iot[0:NE, :], scalar1=cn[0:NE, 0:1], scalar2=None, op0=ALU.is_lt),
             reads=[iotB, cnB], writes=[cmpB])
        s.op("dve", lambda e: e.reduce_sum(out=cn[0:NE, 1:2], in_=cmp_[0:NE, :], axis=AX.X), reads=[cmpB], writes=[cnB])
        s.op("dve", lambda e: e.tensor_scalar(out=cn[0:NE, 2:3], in0=cn[0:NE, 1:2], scalar1=float(BS), scalar2=None, op0=ALU.mult), reads=[cnB], writes=[cnB])
        s.op("pe", lambda e: e.matmul(PS[1][0][0:NE, 0:1], lhsT=une[0:NE, 0, :], rhs=cn[0:NE, 2:3], start=True, stop=True), reads=[uneB, cnB], writes=[PS[1][1]])
        s.op("dve", lambda e: e.tensor_copy(out=cn[0:NE, 3:4], in_=PS[1][0][0:NE, 0:1]), reads=[PS[1][1]], writes=[cnB])
        s.op("dve", lambda e: e.tensor_scalar(out=cmp_[0:NE, :], in0=iot[0:NE, :], scalar1=cn[0:NE, 3:4], scalar2=None, op0=ALU.is_ge),
             reads=[iotB, cnB], writes=[cmpB])
        s.op("pe", lambda e: e.matmul(PS[2][0][:, 0:NB], lhsT=ones_f[0:NE, :], rhs=cmp_[0:NE, :], start=True, stop=True), reads=[cmpB, onesfB], writes=[PS[2][1]])
        s.op("dve", lambda e: e.tensor_scalar(out=bke, in0=PS[2][0][:, 0:NB], scalar1=float(NE - 1), scalar2=float(l * NE), op0=ALU.min, op1=ALU.add),
             reads=[PS[2][1]], writes=[bkeB])
        s.op("dve", lambda e: e.tensor_copy(out=idxE, in_=bke), reads=[bkeB], writes=[idxEB])
        s.op("dve", lambda e: e.tensor_scalar(out=bki, in0=bke, scalar1=128.0, scalar2=pio[:, 0:1], op0=ALU.mult, op1=ALU.add), reads=[bkeB, pioB], writes=[bkiB])
        s.op("dve", lambda e: e.tensor_copy(out=idxW, in_=bki), reads=[bkiB], writes=[idxWB])
        s.op("dve", lambda e: e.tensor_scalar(out=bki, in0=bke, scalar1=float(2 * FF // 128), scalar2=pio[:, 0:1], op0=ALU.mult, op1=ALU.add), reads=[bkeB, pioB, idxWB], writes=[bkiB])
        s.op("dve", lambda e: e.tensor_copy(out=idx16, in_=bki), reads=[bkiB], writes=[idx16B])
        s.op("dve", lambda e: e.tensor_copy(out=pbc[0:NE, :], in_=cn[0:NE, 2:3].to_broadcast([NE, 128])), reads=[cnB], writes=[pbcB])
        s.op("pe", lambda e: e.matmul(PS[3][0][:, 0:NE], lhsT=pbc[0:NE, :], rhs=une[0:NE, 1, :], start=True, stop=True), reads=[pbcB, uneB], writes=[PS[3][1]])
        s.op("dve", lambda e: e.tensor_copy(out=PBR, in_=PS[3][0][:, 0:NE]), reads=[PS[3][1]], writes=[PBRB])
        for j in range(NT):
            b0 = 4 + 2 * (j % 2)
            s.op("pe", lambda e, j=j, b0=b0: e.matmul(PS[b0][0][:, 0:NE], lhsT=ustr, rhs=M_all[:, j, :], start=True, stop=True),
                 reads=[ustrB, M_allB], writes=[PS[b0][1]])
            s.op("pe", lambda e, j=j, b0=b0: e.matmul(PS[b0 + 1][0][:, 0:NE], lhsT=ones_f, rhs=M_all[:, j, :], start=True, stop=True),
                 reads=[onesfB, M_allB], writes=[PS[b0 + 1][1]])
            s.op("dve", lambda e, b0=b0: e.tensor_tensor(out=dj, in0=PS[b0][0][:, 0:NE], in1=PBR, op=ALU.add), reads=[PS[b0][1], PBRB], writes=[djB])
            s.op("dve", lambda e, b0=b0: e.tensor_tensor(out=PBR, in0=PS[b0 + 1][0][:, 0:NE], in1=PBR, op=ALU.add), reads=[PS[b0 + 1][1], PBRB], writes=[PBRB])
            for k in range(4):
                s.op("dve", lambda e, j=j, k=k: e.tensor_scalar(out=tq, in0=L_all[:, j, :], scalar1=M4[:, j, k:k + 1], scalar2=None, op0=ALU.is_equal),
                     reads=[L_allB, M4B], writes=[tqB])
                s.op("dve", lambda e: e.tensor_tensor(out=tq, in0=tq, in1=dj, op=ALU.mult), reads=[tqB, djB], writes=[tqB])
                s.op("dve", lambda e, j=j, k=k: e.reduce_sum(out=D4[:, j, k:k + 1], in_=tq, axis=AX.X), reads=[tqB], writes=[D4B])
        s.op("dve", lambda e: e.tensor_scalar(out=D4, in0=D4, scalar1=0.0, scalar2=float(NR - 1), op0=ALU.max, op1=ALU.min), reads=[D4B], writes=[D4B])
        s.op("dve", lambda e: e.tensor_copy(out=D4i, in_=D4), reads=[D4B], writes=[D4iB])
        s.barrier()
        B.reset_arena(KEEP2)
        hp = B.pool("hsc", [D], BF16, 3)
        for j in range(NT):
            hb, hbB = hp[j % 3]
            s.op("sp", lambda e, hb=hb, j=j: e.dma_start(out=hb, in_=hfb.ap()[j * 128:(j + 1) * 128, :]), reads=[db("hfb")], writes=[hbB], dma_buf=hbB)
            for k in range(4):
                s.op("pool", lambda e, hb=hb, j=j, k=k: e.indirect_dma_start(
                    out=xsr.ap()[0:NR, :], out_offset=bass.IndirectOffsetOnAxis(ap=D4i[:, j, k:k + 1], axis=0),
                    in_=hb, in_offset=None),
                    reads=[hbB, D4iB], writes=[db("xsr")], dma_buf=hbB)
        s.barrier()
        B.reset_arena(KEEP2)
        Wg = B.pool("Wg", [8, 2 * FF], BF16, 2)
        Wd = B.pool("Wd", [NFC, D], BF16, 2)
        bgt = B.pool("bgt", [128], F32, 2)
        bcl = B.pool("bcl", [2 * NFC], F32, 2)
        bdb = B.pool("bdb", [D], F32, 2)
        xb = B.pool("xb", [4, D], BF16, 2)
        xT, xTB = B.sb("xT", [8, 512], BF16)
        gg, ggB = B.sb("gg", [512], F32)
        sgm, sgmB = B.sb("sgm", [512], F32)
        uc, ucB = B.sb("uc", [512], F32)
        yT, yTB = B.sb("yT", [NFC, 512], BF16)
        yot, yoB = B.sb("yo", [4, D], F32)
        NG = 2 * NFC
        for b in range(NB):
            wg, wgB = Wg[b % 2]; wd, wdB = Wd[b % 2]; bg, bgB = bgt[b % 2]; bc_, bcB = bcl[b % 2]; bd, bdB = bdb[b % 2]
            s.op("pool", lambda e, wg=wg, b=b: e.indirect_dma_start(out=wg.rearrange("p a b -> p (a b)"), out_offset=None, in_=wgb.ap(),
                                                                  in_offset=bass.IndirectOffsetOnAxis(ap=idxW[:, b:b + 1], axis=0)),
                 reads=[idxWB, db("wgb")], writes=[wgB], dma_buf=wgB)
            s.op("pool", lambda e, wd=wd, b=b: e.indirect_dma_start(out=wd.rearrange("p a b -> p (a b)"), out_offset=None, in_=wdb.ap(),
                                                                  in_offset=bass.IndirectOffsetOnAxis(ap=idxW[:, b:b + 1], axis=0)),
                 reads=[idxWB, db("wdb")], writes=[wdB], dma_buf=wdB)
            s.op("pool", lambda e, bg=bg, b=b: e.indirect_dma_start(out=bg[0:NG, :], out_offset=None, in_=b_gu.ap(),
                                                                  in_offset=bass.IndirectOffsetOnAxis(ap=idx16[0:NG, b:b + 1], axis=0)),
                 reads=[idx16B], writes=[bgB], dma_buf=bgB)
            s.op("pool", lambda e, bd=bd, b=b: e.indirect_dma_start(out=bd, out_offset=None, in_=b_dn.ap(),
                                                                  in_offset=bass.IndirectOffsetOnAxis(ap=idxE[:, b:b + 1], axis=0)),
                 reads=[idxEB], writes=[bdB], dma_buf=bdB)
            xbt, xbB = xb[b % 2]
            s.op("sp", lambda e, xbt=xbt, b=b: e.dma_start(out=xbt, in_=xsr.ap()[b * BS:(b + 1) * BS, :].rearrange("(r p) c -> p r c", p=128)),
                 reads=[db("xsr")], writes=[xbB], dma_buf=xbB)
            s.op("pe", lambda e, bg=bg: e.transpose(out=PS[6][0][:, 0:NG], in_=bg[0:NG, :], identity=id32[0:NG, 0:NG]), reads=[bgB, id32B], writes=[PS[6][1]])
            s.op("act", lambda e, bc_=bc_: e.copy(out=bc_, in_=PS[6][0][:, 0:NG]), reads=[PS[6][1]], writes=[bcB])
            s.op("dve", lambda e, bc_=bc_: e.tensor_scalar(out=bc_[:, NFC:NG], in0=bc_[:, NFC:NG], scalar1=1.0, scalar2=None, op0=ALU.add), reads=[bcB], writes=[bcB])
            for k2 in range(4):
                bank = k2 % 2

                def ftx(e, xbt=xbt, k2=k2, bank=bank):
                    pv = psb(bank)
                    for kk in range(2):
                        for r in range(4):
                            k = 2 * k2 + kk
                            ins = e.transpose(out=pv[:, (kk * 4 + r) * 128:(kk * 4 + r + 1) * 128], in_=xbt[:, r, k * 128:(k + 1) * 128], identity=idb)
                    return ins
                s.op("pe", ftx, reads=[xbB, idbB], writes=[PS[bank][1]])
                s.op("act", lambda e, k2=k2, bank=bank: e.copy(out=xT[:, 2 * k2:2 * k2 + 2, :].rearrange("p a b -> p (a b)"), in_=psb(bank)),
                     reads=[PS[bank][1]], writes=[xTB])
            for i in range(NFC):
                bgk, buk = 2 + 2 * (i % 2), 3 + 2 * (i % 2)

                def fgu(e, wg=wg, i=i, bgk=bgk, buk=buk):
                    for bank, fc in ((bgk, i), (buk, NFC + i)):
                        for k in range(8):
                            ins = e.matmul(PS[bank][0], lhsT=wg[:, k, fc * 128:(fc + 1) * 128], rhs=xT[:, k, :], start=(k == 0), stop=(k == 7))
                    return ins
                s.op("pe", fgu, reads=[wgB, xTB], writes=[PS[bgk][1], PS[buk][1]])
                s.op("dve", lambda e, bgk=bgk, bc_=bc_, i=i: e.tensor_scalar(out=gg, in0=PS[bgk][0], scalar1=bc_[:, i:i + 1], scalar2=7.0, op0=ALU.add, op1=ALU.min),
                     reads=[PS[bgk][1], bcB], writes=[ggB])
                s.op("act", lambda e: e.activation(out=sgm, in_=gg, func=AF.Sigmoid, scale=1.702), reads=[ggB], writes=[sgmB])
                s.op("dve", lambda e: e.tensor_tensor(out=sgm, in0=sgm, in1=gg, op=ALU.mult), reads=[sgmB, ggB], writes=[sgmB])
                s.op("dve", lambda e, buk=buk, bc_=bc_, i=i: e.tensor_scalar(out=uc, in0=PS[buk][0], scalar1=bc_[:, NFC + i:NFC + i + 1], scalar2=-6.0, op0=ALU.add, op1=ALU.max),
                     reads=[PS[buk][1], bcB], writes=[ucB])
                s.op("dve", lambda e, i=i: e.scalar_tensor_tensor(out=yT[:, i, :], in0=uc, scalar=8.0, in1=sgm, op0=ALU.min, op1=ALU.mult),
                     reads=[ucB, sgmB], writes=[yTB])
            for rt in range(4):
                for dc in range(2):
                    bank = 6 + (rt * 2 + dc) % 2

                    def fdn(e, wd=wd, rt=rt, dc=dc, bank=bank):
                        for k in range(NFC):
                            ins = e.matmul(PS[bank][0], lhsT=yT[:, k, rt * 128:(rt + 1) * 128], rhs=wd[:, k, dc * 512:(dc + 1) * 512], start=(k == 0), stop=(k == NFC - 1))
                        return ins
                    s.op("pe", fdn, reads=[wdB, yTB], writes=[PS[bank][1]])
                    s.op("dve", lambda e, bd=bd, rt=rt, dc=dc, bank=bank: e.tensor_tensor(out=yot[:, rt, dc * 512:(dc + 1) * 512], in0=PS[bank][0],
                                                                                         in1=bd[:, dc * 512:(dc + 1) * 512], op=ALU.add),
                         reads=[PS[bank][1], bdB, yoB], writes=[yoB])
            s.op("sp", lambda e, b=b: e.dma_start(out=ysr.ap()[b * BS:(b + 1) * BS, :].rearrange("(r p) c -> p r c", p=128), in_=yot),
                 reads=[yoB], writes=[db("ysr")], dma_buf=yoB)
        s.barrier()
        B.reset_arena(KEEP2)
        gfb = [B.sb("gfb%d" % i, [D], F32) for i in range(2)]
        for g_ in range(2):
            load_bc("sp", gfb[g_][0], gfb[g_][1], modv.ap()[l, g_, 5:6, :])
        yk = B.pool("yk", [D], F32, 4)
        xq = B.pool("xq", [D], F32, 2)
        ac = B.pool("acm", [D], F32, 2)
        for j in range(NT):
            x, xB = xq[j % 2]
            s.op("sp", lambda e, x=x, j=j: e.dma_start(out=x, in_=src.ap()[j * 128:(j + 1) * 128, :]), reads=[db(srcname)], writes=[xB], dma_buf=xB)
            a_, aB = ac[j % 2]
            for k in range(4):
                y_, yB = yk[k]
                s.op("pool", lambda e, y_=y_, j=j, k=k: e.indirect_dma_start(
                    out=y_, out_offset=None, in_=ysr.ap()[0:NR, :], in_offset=bass.IndirectOffsetOnAxis(ap=D4i[:, j, k:k + 1], axis=0)),
                    reads=[db("ysr"), D4iB], writes=[yB], dma_buf=yB)
                if k == 0:
                    s.op("dve", lambda e, y_=y_, a_=a_, j=j: e.tensor_scalar(out=a_, in0=y_, scalar1=G4[:, j, 0:1], scalar2=None, op0=ALU.mult),
                         reads=[yB, G4B], writes=[aB])
                else:
                    s.op("dve", lambda e, y_=y_, a_=a_, j=j, k=k: e.scalar_tensor_tensor(out=a_, in0=y_, scalar=G4[:, j, k:k + 1], in1=a_, op0=ALU.mult, op1=ALU.add),
                         reads=[yB, G4B, aB], writes=[aB])
            g_ = grp_of_tile(j)
            s.op("dve", lambda e, a_=a_, g_=g_: e.tensor_tensor(out=a_, in0=a_, in1=gfb[g_][0], op=ALU.mult), reads=[aB, gfb[g_][1]], writes=[aB])
            s.op("dve", lambda e, a_=a_, x=x: e.tensor_tensor(out=a_, in0=a_, in1=x, op=ALU.add), reads=[aB, xB], writes=[aB])
            s.op("sp", lambda e, a_=a_, j=j: e.dma_start(out=dst.ap()[j * 128:(j + 1) * 128, :], in_=a_), reads=[aB], writes=[db(dstname)], dma_buf=aB)
        s.barrier()

    B.reset_arena(KEEP)
    stg = B.pool("stg", [8, 2 * FF], BF16, 2)
    std = B.pool("std", [NFC, D], BF16, 2)
    for ee in range(2 * NE):
        sg_, sgB_ = stg[ee % 2]
        s.op("pool", lambda e, sg_=sg_, ee=ee: e.dma_start(out=sg_, in_=w_gu.ap()[ee].rearrange("(k p) c -> p k c", p=128)), writes=[sgB_], dma_buf=sgB_)
        s.op("sp", lambda e, sg_=sg_, ee=ee: e.dma_start(out=wgb.ap()[ee * 128:(ee + 1) * 128, :], in_=sg_.rearrange("p a b -> p (a b)")),
             reads=[sgB_], writes=[db("wgb")], dma_buf=sgB_)
        sd_, sdB_ = std[ee % 2]
        s.op("pool", lambda e, sd_=sd_, ee=ee: e.dma_start(out=sd_, in_=w_dn.ap()[ee].rearrange("(k p) c -> p k c", p=128)), writes=[sdB_], dma_buf=sdB_)
        s.op("sp", lambda e, sd_=sd_, ee=ee: e.dma_start(out=wdb.ap()[ee * 128:(ee + 1) * 128, :], in_=sd_.rearrange("p a b -> p (a b)")),
             reads=[sdB_], writes=[db("wdb")], dma_buf=sdB_)
    s.barrier()
    if cfg.stop_after == "PW":
        return finish(B, cfg)

    moe(0, x1, "x1", cfg.ET // 128, lambda j: 0 if j < E[0] // 128 else 1, x2, "x2")
    if cfg.stop_after == "P3":
        return finish(B, cfg)

    def proj1_group(g):
        d = DIL[g]
        B.reset_arena(KEEP)
        Wb, WbB = B.sb("Wb", [8, 3072], BF16)
        load_w_bf16(Wb, WbB, w_qkv_b.ap(), g * 3072, 3072)
        gq1, gq1B = B.sb("gq1", [64], F32); gk1, gk1B = B.sb("gk1", [64], F32)
        load_bc("sp", gq1, gq1B, q_norm_b.ap()[g:g + 1, :]); load_bc("sp", gk1, gk1B, k_norm_b.ap()[g:g + 1, :])
        s.op("dve", lambda e: e.tensor_scalar(out=gq1, in0=gq1, scalar1=0.125, scalar2=None, op0=ALU.mult), reads=[gq1B], writes=[gq1B])
        bcm1 = [B.sb("bcm1_%d" % i, [2, D], F32) for i in range(2)]
        for sg in range(2):
            load_bc("sp", bcm1[sg][0][:, 0, :], bcm1[sg][1], modv.ap()[1, sg, 1:2, :])
            load_bc("sp", bcm1[sg][0][:, 1, :], bcm1[sg][1], modv.ap()[1, sg, 0:1, :])
        xt = B.pool("xt1", [D], F32, 2)
        sq, sqB = B.sb("sq1", [D], F32); ss, ssB = B.sb("ss1", [1], F32); t32, t32B = B.sb("t321", [D], F32)
        hm = B.pool("hm1", [D], BF16, 2)
        hTn = B.pool("hTn", [8, 128], BF16, 2)
        hTr = B.pool("hTr", [8, 128], BF16, 2)
        pf, pfB = B.sb("pf", [3072], F32)
        pn, pnB = B.sb("pn", [2048], F32)
        ssh, sshB = B.sb("ssh1", [32], F32)
        qkb = B.pool("qkb", [2048], BF16, 2)
        vxt = B.pool("vxt1", [16, 65], BF16, 2)
        for i in range(2):
            s.op("dve", lambda e, i=i: e.memset(vxt[i][0], 1.0), writes=[vxt[i][1]])
        qkT = B.pool("qkT", [16, 128], BF16, 2)
        it_ = 0
        for sg in range(2):
            NIg = NI[g][sg]
            base = segoff[sg] + H - 64 * d
            for r in range(d):
                for tl in range(NIg // 128):
                    x, xB = xt[it_ % 2]
                    row0 = base + r + d * 128 * tl
                    s.op("sp", lambda e, x=x, row0=row0, d=d: e.dma_start(out=x, in_=bass.AP(x2, row0 * D, [[d * D, 128], [1, D]])),
                         reads=[db("x2")], writes=[xB], dma_buf=xB)
                    hmb, hmbB = hm[it_ % 2]
                    norm_tile(x, xB, bcm1[sg][0][:, 0, :], bcm1[sg][0][:, 1, :], bcm1[sg][1], sq, sqB, ss, ssB, t32, t32B, hmb, hmbB)
                    hn, hnB = hTn[it_ % 2]; hr, hrB = hTr[it_ % 2]
                    transpose8(hmb, hmbB, idb, idbB, 0, hn, hnB, eng="act")
                    transpose8(hmb, hmbB, antib, antibB, 1, hr, hrB, eng="dve")
                    for cc in range(6):
                        src_T, src_TB = (hn, hnB) if cc < 2 else (hr, hrB)
                        proj(src_T, src_TB, Wb, WbB, cc * 512, 512, 2 + cc)
                    for cc in range(6):
                        s.op("act", lambda e, cc=cc: e.copy(out=pf[:, cc * 512:(cc + 1) * 512], in_=PS[2 + cc][0]), reads=[PS[2 + cc][1], pfB], writes=[pfB])
                    headnorm(pf[:, 0:1024], pfB, 16, gq1, gq1B, sq, sqB, ssh, sshB, pn[:, 0:1024], pnB)
                    headnorm(pf[:, 1024:2048], pfB, 16, gk1, gk1B, sq, sqB, ssh, sshB, pn[:, 1024:2048], pnB)
                    qk, qkB = qkb[it_ % 2]
                    s.op("act", lambda e, qk=qk: e.copy(out=qk, in_=pn), reads=[pnB], writes=[qkB])
                    vt, vtB = vxt[it_ % 2]
                    s.op("dve", lambda e, vt=vt: e.tensor_copy(out=vt[:, :, 0:64], in_=pf[:, 2048:3072].rearrange("p (h c) -> p h c", c=64)),
                         reads=[pfB], writes=[vtB])
                    col0 = r * NIg + tl * 128
                    s.op("sp", lambda e, vt=vt, col0=col0, g=g, sg=sg: e.dma_start(out=vx1[g][sg].ap()[col0:col0 + 128, :, :], in_=vt),
                         reads=[vtB], writes=[db("vx1_%d_%d" % (g, sg))], dma_buf=vtB)
                    qT_, qT_B = qkT[it_ % 2]

                    def ftt(e, qk=qk):
                        for half in range(2):
                            pv = psb(half)
                            for pp in range(8):
                                c0 = half * 1024 + pp * 128
                                ins = e.transpose(out=pv[:, pp * 128:(pp + 1) * 128], in_=qk[:, c0:c0 + 128], identity=idb)
                        return ins
                    s.op("pe", ftt, reads=[qkB, idbB], writes=[PS[0][1], PS[1][1]])
                    s.op("act", lambda e, qT_=qT_: e.copy(out=qT_[:, 0:8, :].rearrange("p a b -> p (a b)"), in_=psb(0)), reads=[PS[0][1]], writes=[qT_B])
                    s.op("dve", lambda e, qT_=qT_: e.tensor_copy(out=qT_[:, 8:16, :].rearrange("p a b -> p (a b)"), in_=psb(1)), reads=[PS[1][1], qT_B], writes=[qT_B])
                    s.op("sp", lambda e, qT_=qT_, col0=col0, g=g, sg=sg: e.dma_start(out=qT1[g][sg].ap()[:, :, col0:col0 + 128], in_=qT_[:, 0:8, :]),
                         reads=[qT_B], writes=[db("qT1_%d_%d" % (g, sg))], dma_buf=qT_B)
                    s.op("sp", lambda e, qT_=qT_, col0=col0, g=g, sg=sg: e.dma_start(out=kT1[g][sg].ap()[:, :, col0:col0 + 128], in_=qT_[:, 8:16, :]),
                         reads=[qT_B], writes=[db("kT1_%d_%d" % (g, sg))], dma_buf=qT_B)
                    it_ += 1
        s.barrier()
    for g_i in range(3):
        proj1_group(g_i)
    if cfg.stop_after == "P4":
        return finish(B, cfg)

    if DEBUG.get("skip_to_p5a"):
        for e_ in ENGS:
            s.ops[e_] = []
        for b_ in s.phase_bufs + [p_[1] for p_ in PS]:
            b_.w = []; b_.r = []; b_.dsem = None
        s.dsem_count = [0] * N_DSEM
        s.dsem_next = 0
        s.dma_since_barrier = {}
    B.reset_arena(KEEP)
    tab, tabB = B.sb("tab", [48], F32)
    s.op("dve", lambda e: e.memset(tab[32:64, :], 1.0), writes=[tabB])
    s.op("sp", lambda e: e.dma_start(out=tab[0:32, :], in_=rel_bias.ap()), writes=[tabB], dma_buf=tabB)
    oh, ohB = B.sb("oh", [6, 256], F32)
    s.op("sp", lambda e: e.dma_start(out=oh[0:33], in_=c_oh.ap().rearrange("g a k m -> k (g a) m")), writes=[ohB], dma_buf=ohB)
    fvt, fvtB = B.sb("fvt", [6, 256], F32)
    for g in range(3):
        for ab in range(2):
            def ffv(e, g=g, ab=ab):
                return e.matmul(PS[0][0][0:16, 0:256], lhsT=tab[0:33, g * 16:(g + 1) * 16], rhs=oh[0:33, g * 2 + ab, :], start=True, stop=True)
            s.op("pe", ffv, reads=[tabB, ohB, onesfB], writes=[PS[0][1]])
            s.op("dve", lambda e, g=g, ab=ab: e.tensor_copy(out=fvt[0:16, g * 2 + ab, :], in_=PS[0][0][0:16, 0:256]), reads=[PS[0][1]], writes=[fvtB])
    s.op("sp", lambda e: e.dma_start(out=fvs.ap().rearrange("g a h m -> h (g a) m"), in_=fvt[0:16]), reads=[fvtB], writes=[db("fvs")], dma_buf=fvtB)
    BM, BMB = B.sb("BM", [3, 2, 16, 128], F32)
    for g in range(3):
        for ab in range(2):
            for hh in range(16):
                s.op("sp", lambda e, g=g, ab=ab, hh=hh: e.dma_start(out=BM[:, g, ab, hh, :],
                                                                 in_=bass.AP(fvs, ((g * 2 + ab) * 16 + hh) * 256, [[1, 128], [1, 128]])),
                     reads=[db("fvs")], writes=[BMB], dma_buf=BMB)
    km, kmB = B.sb("km", [NKM], F32)
    s.op("sp", lambda e: e.dma_start(out=km, in_=kmaskT.ap()), writes=[kmB], dma_buf=kmB)
    if cfg.stop_after == "P5a0":
        s.barrier()
        return finish(B, cfg)
    KTl = B.pool("KTl", [8, 256], BF16, 2)
    QTl = B.pool("QTl", [8, 128], BF16, 2)
    VXl = B.pool("VXl", [2, 16, 65], BF16, 2)
    sc, scB = B.sb("sc", [2, 4, 128], F32)
    pT1 = B.pool("pT1", [2, 4, 128], BF16, 2)
    numt = B.pool("numt", [16, 65], F32, 2)
    kcol = 0
    it_ = 0
    ownoff = (0, T[0])
    for g in range(3):
        d = DIL[g]
        for sg in range(2):
            NIg = NI[g][sg]
            nqt = T[sg] // (128 * d)
            for r in range(d):
                for qt in range(nqt):
                    c0 = r * NIg + qt * 128
                    KTt, KTtB = KTl[it_ % 2]; QTt, QTtB = QTl[it_ % 2]; VXt, VXtB = VXl[it_ % 2]
                    s.op("sp", lambda e, KTt=KTt, c0=c0, g=g, sg=sg: e.dma_start(out=KTt, in_=kT1[g][sg].ap()[:, :, c0:c0 + 256]),
                         reads=[db("kT1_%d_%d" % (g, sg))], writes=[KTtB], dma_buf=KTtB)
                    s.op("sp", lambda e, QTt=QTt, c0=c0, g=g, sg=sg: e.dma_start(out=QTt, in_=qT1[g][sg].ap()[:, :, c0 + 64:c0 + 192]),
                         reads=[db("qT1_%d_%d" % (g, sg))], writes=[QTtB], dma_buf=QTtB)
                    s.op("sp", lambda e, VXt=VXt, c0=c0, g=g, sg=sg: e.dma_start(out=VXt, in_=vx1[g][sg].ap()[c0:c0 + 256, :, :].rearrange("(a k) h c -> k a h c", k=128)),
                         reads=[db("vx1_%d_%d" % (g, sg))], writes=[VXtB], dma_buf=VXtB)
                    nt, ntB = numt[it_ % 2]
                    kc = kcol + r * (NIg // 128) + qt
                    STG = DEBUG.get("p5a_stage", 9)
                    if it_ >= DEBUG.get("p5a_max_it", 10 ** 9):
                        it_ += 1
                        continue
                    for hq in range(4 if STG >= 2 else 0):
                        bs_ = 2 * (hq % 2)

                        def fsc(e, hq=hq, bs_=bs_, KTt=KTt, QTt=QTt):
                            for u in range(2):
                                for ab in range(2):
                                    for hx in range(2):
                                        hh = hq * 4 + 2 * hx + u
                                        pp = hh // 2
                                        cb = (ab * 2 + hx) * 128
                                        ins = e.matmul(PS[bs_ + u][0][:, cb:cb + 128], lhsT=KTt[64 * u:64 * u + 64, pp, ab * 128:(ab + 1) * 128],
                                                       rhs=QTt[64 * u:64 * u + 64, pp, :], start=True, stop=True)
                            return ins
                        s.op("pe", fsc, reads=[KTtB, QTtB], writes=[PS[bs_][1], PS[bs_ + 1][1]])
                        if STG < 3:
                            continue
                        for u in range(2):
                            for ab in range(2):
                                s.op("dve", lambda e, hq=hq, ab=ab, u=u, bs_=bs_, g=g, kc=kc: e.scalar_tensor_tensor(
                                    out=sc[:, ab, u:4:2, :], in0=PS[bs_ + u][0][:, ab * 256:(ab + 1) * 256].rearrange("p (a b) -> p a b", a=2),
                                    scalar=km[:, kc + ab:kc + ab + 1], in1=BM[:, g, ab, hq * 4 + u:hq * 4 + 4:2, :], op0=ALU.add, op1=ALU.add),
                                    reads=[PS[bs_ + u][1], kmB, BMB, scB], writes=[scB])
                        pT, pTB = pT1[hq % 2]
                        if STG < 4:
                            continue
                        s.op("act", lambda e, pT=pT: e.activation(out=pT.rearrange("p a b c -> p (a b c)"), in_=sc.rearrange("p a b c -> p (a b c)"), func=AF.Exp),
                             reads=[scB], writes=[pTB])
                        ob = 4 + (hq % 2)
                        if STG < 5:
                            continue

                        def fpv(e, hq=hq, pT=pT, VXt=VXt, ob=ob):
                            for hi_ in range(4):
                                hh = hq * 4 + hi_
                                for ab in range(2):
                                    ins = e.matmul(PS[ob][0][:, hi_ * 65:(hi_ + 1) * 65], lhsT=pT[:, ab, hi_, :], rhs=VXt[:, ab, hh, :], start=(ab == 0), stop=(ab == 1))
                            return ins
                        s.op("pe", fpv, reads=[pTB, VXtB], writes=[PS[ob][1]])
                        if STG < 6:
                            continue
                        s.op("act", lambda e, nt=nt, hq=hq, ob=ob: e.copy(out=nt[:, hq * 4:hq * 4 + 4, :].rearrange("p a b -> p (a b)"), in_=PS[ob][0][:, 0:260]),
                             reads=[PS[ob][1], ntB], writes=[ntB])
                    row0 = ownoff[sg] + r + d * 128 * qt
                    if STG < 7:
                        it_ += 1
                        continue
                    s.op("sp", lambda e, nt=nt, row0=row0, g=g, d=d: e.dma_start(out=bass.AP(num1[g], row0 * 1040, [[d * 1040, 128], [1, 1040]]),
                                                                              in_=nt.rearrange("p a b -> p (a b)")),
                         reads=[ntB], writes=[db("num1_%d" % g)], dma_buf=ntB)
                    it_ += 1
            kcol += d * (NIg // 128)
    s.barrier()
    if cfg.stop_after == "P5a":
        return finish(B, cfg)

    oT1 = B.dscr("oT1", [64, 16, cfg.TT], BF16)
    B.reset_arena(KEEP)
    nm = B.pool("nm", [3, 16, 65], F32, 2)
    rcd, rcdB = B.sb("rcd", [16], F32)
    ob16 = B.pool("ob16", [D], BF16, 2)
    oTs = B.pool("oTs", [16, 128], BF16, 2)
    for j in range(cfg.TT // 128):
        n_, nB = nm[j % 2]
        for g in range(3):
            s.op("sp", lambda e, n_=n_, g=g, j=j: e.dma_start(out=n_[:, g, :, :], in_=num1[g].ap()[j * 128:(j + 1) * 128, :, :]),
                 reads=[db("num1_%d" % g)], writes=[nB], dma_buf=nB)
        s.op("dve", lambda e, n_=n_: e.tensor_tensor(out=n_[:, 0, :, :], in0=n_[:, 0, :, :], in1=n_[:, 1, :, :], op=ALU.add), reads=[nB], writes=[nB])
        s.op("dve", lambda e, n_=n_: e.tensor_tensor(out=n_[:, 0, :, :], in0=n_[:, 0, :, :], in1=n_[:, 2, :, :], op=ALU.add), reads=[nB], writes=[nB])
        s.op("dve", lambda e, n_=n_: e.reciprocal(out=rcd, in_=n_[:, 0, :, 64]), reads=[nB], writes=[rcdB])
        o_, oB = ob16[j % 2]
        s.op("dve", lambda e, n_=n_, o_=o_: e.tensor_tensor(out=o_.rearrange("p (h c) -> p h c", c=64), in0=n_[:, 0, :, 0:64],
                                                         in1=rcd.unsqueeze(2).to_broadcast([128, 16, 64]), op=ALU.mult), reads=[nB, rcdB], writes=[oB])
        oT_, oT_B = oTs[j % 2]
        bank = j % 2

        def fto(e, o_=o_, bank=bank):
            pv = psb(bank)
            for hh in range(16):
                ins = e.transpose(out=pv[0:64, hh * 64:(hh + 1) * 64].rearrange("p c -> p c"), in_=o_[:, hh * 64:(hh + 1) * 64], identity=idb)
            return ins
        def fto2(e, o_=o_, bank=bank):
            for half in range(2):
                pv = psb(2 * bank + half)
                for hh in range(8):
                    ins = e.transpose(out=pv[0:64, hh * 128:(hh + 1) * 128], in_=o_[:, (half * 8 + hh) * 64:(half * 8 + hh + 1) * 64], identity=idb)
            return ins
        s.op("pe", fto2, reads=[oB, idbB], writes=[PS[2 * bank][1], PS[2 * bank + 1][1]])
        s.op("act", lambda e, oT_=oT_, bank=bank: e.copy(out=oT_[0:64, 0:8, :].rearrange("p a b -> p (a b)"), in_=psb(2 * bank)[0:64, :]), reads=[PS[2 * bank][1]], writes=[oT_B])
        s.op("act", lambda e, oT_=oT_, bank=bank: e.copy(out=oT_[0:64, 8:16, :].rearrange("p a b -> p (a b)"), in_=psb(2 * bank + 1)[0:64, :]), reads=[PS[2 * bank + 1][1], oT_B], writes=[oT_B])
        s.op("sp", lambda e, oT_=oT_, j=j: e.dma_start(out=oT1.ap()[:, :, j * 128:(j + 1) * 128], in_=oT_[0:64]), reads=[oT_B], writes=[db("oT1")], dma_buf=oT_B)
    s.barrier()

    def x2_loader(x, xB, sg, jq):
        row = segoff[sg] + H + jq * 128
        s.op("sp", lambda e, x=x, row=row: e.dma_start(out=x, in_=x2.ap()[row:row + 128, :]), reads=[db("x2")], writes=[xB], dma_buf=xB)

    wo_phase(1, lambda sg, jq: oT1.ap()[:, :, ownoff[sg] + jq * 128:ownoff[sg] + (jq + 1) * 128], [db("oT1")], x2_loader,
             lambda sg: sg, [T[0] // 128, T[1] // 128], x3, "x3", w_o_b)
    if cfg.stop_after == "P5":
        return finish(B, cfg)
    moe(1, x3, "x3", cfg.TT // 128, lambda j: 0 if j < T[0] // 128 else 1, yout, "y")
    return finish(B, cfg)


def finish(B, cfg):
    B.s.op("sp", None, reads=[B.dbuf[n] for n in B.dbuf])
    B.s.emit(B.nc, B.stack)
    B.stack.close()
    return B.nc


def _t5_bucket(rel):
    nb = 16
    max_exact = 8
    ret = (rel > 0).astype(np.int32) * nb
    n = np.abs(rel)
    large = max_exact + (np.log(np.maximum(n, 1) / max_exact) / np.log(1024 / max_exact) * (nb - max_exact)).astype(np.int32)
    large = np.minimum(large, nb - 1)
    return (ret + np.where(n < max_exact, n, large)).astype(np.int32)


def _consts(cfg):
    c = {}
    c["c_ident"] = np.eye(128, dtype=np.float32)
    c["c_anti"] = np.eye(128, dtype=np.float32)[::-1].copy()
    c["c_ustrict"] = np.triu(np.ones((128, 128), np.float32), 1)
    NE = cfg.NE
    c["c_une"] = np.stack([np.triu(np.ones((NE, NE), np.float32), 0), np.triu(np.ones((NE, NE), np.float32), 1)])
    NB = cfg.nblk(cfg.ET)
    c["c_iotab"] = np.tile((np.arange(NB, dtype=np.float32) * BS)[None, :], (NE, 1))
    c["c_piota"] = np.arange(128, dtype=np.float32).reshape(128, 1)
    SP0 = cfg.S[0] + 2 * H
    t = np.arange(SP0, dtype=np.float64) - H
    row = np.floor(t / 64.0).astype(np.float32)
    col = (t - np.floor(t / 64.0) * 64).astype(np.float32)
    inv = (1.0 / (np.float32(10000.0) ** (np.arange(16, dtype=np.float32) / np.float32(16)))).astype(np.float32)
    ar = row[:, None] * inv[None, :]
    ac = col[:, None] * inv[None, :]
    ang = np.concatenate([ar, ar, ac, ac], axis=-1).astype(np.float32)
    sgn = np.concatenate([-np.ones(16), np.ones(16), -np.ones(16), np.ones(16)]).astype(np.float32)
    c["ropep"] = np.concatenate([np.cos(ang), np.sin(ang) * sgn[None, :]], axis=-1).astype(np.float32)
    oh = np.zeros((3, 2, 33, 256), np.float32)
    for g, d in enumerate(DIL):
        for ab, off in enumerate((63, 191)):
            m = np.arange(256)
            delta = off - m
            ok = np.abs(delta) <= 64
            bk = _t5_bucket(delta * d)
            for mm in range(256):
                if ok[mm]:
                    oh[g, ab, bk[mm], mm] = 1.0
                else:
                    oh[g, ab, 32, mm] = NEG
    c["c_oh"] = oh
    return c


def _kmask(cfg, q):
    cols = []
    for g, d in enumerate(DIL):
        for sg in range(2):
            Tn, Sn = cfg.T[sg], cfg.S[sg]
            NI = Tn // d + 128
            L = Sn // d
            for r in range(d):
                for tl in range(NI // 128):
                    a = np.arange(128)
                    ik = tl * 128 + (127 - a)
                    gi = (q * Tn) // d + ik - 64
                    cols.append(np.where((gi >= 0) & (gi < L), 0.0, NEG).astype(np.float32))
    return np.stack(cols, axis=1)


def make_in_maps(cfg, inp):
    consts = _consts(cfg)
    maps = []
    NE, FF = cfg.NE, cfg.FF
    shared = {
        "norm_mix_g": inp["norm_mix_g"], "norm_ffn_g": inp["norm_ffn_g"], "w_ada": inp["w_ada"], "b_ada": inp["b_ada"],
        "w_qkv_a": inp["w_qkv_a"][0], "q_norm_a": inp["q_norm_a"], "k_norm_a": inp["k_norm_a"], "w_o_a": inp["w_o_a"][0],
        "w_qkv_b": inp["w_qkv_b"][0], "q_norm_b": inp["q_norm_b"][0], "k_norm_b": inp["k_norm_b"][0], "w_o_b": inp["w_o_b"][0],
        "rel_bias": inp["rel_bias"], "w_router": inp["w_router"], "b_router": inp["b_router"],
        "w_gate_up": inp["w_gate_up"].reshape(2 * NE, D, 2 * FF), "b_gate_up": inp["b_gate_up"].reshape(2 * NE * (2 * FF // 128), 128),
        "w_down": inp["w_down"].reshape(2 * NE, FF, D), "b_down": inp["b_down"].reshape(2 * NE, D),
    }
    shared = {k: np.ascontiguousarray(v, dtype=np.float32) for k, v in shared.items()}
    ropep = consts.pop("ropep")
    for c in range(8):
        b, q = c // 4, c % 4
        m = dict(shared)
        m.update(consts)
        m["xs0"] = np.ascontiguousarray(inp["x_sample"][b], dtype=np.float32)
        m["xs1"] = np.ascontiguousarray(inp["x_prompt"][b], dtype=np.float32)
        xwl, rwl = [], []
        for sg, key in enumerate(("x_sample", "x_prompt")):
            Tn, Sn, En = cfg.T[sg], cfg.S[sg], cfg.E[sg]
            w = np.zeros((En, D), np.float32)
            lo = q * Tn - H
            a, bnd = max(lo, 0), min(lo + En, Sn)
            w[a - lo:bnd - lo] = inp[key][b][a:bnd]
            xwl.append(w)
            rwl.append(ropep[q * Tn:q * Tn + En])
        m["xw"] = np.concatenate(xwl, axis=0)
        m["ropew"] = np.ascontiguousarray(np.concatenate(rwl, axis=0))
        m["ropek"] = np.ascontiguousarray(ropep[H:H + cfg.S[0]])
        m["cvec"] = np.stack([inp["c_sample"][b], inp["c_prompt"][b]]).astype(np.float32)
        m["kmaskT"] = _kmask(cfg, q)
        maps.append(m)
    return maps


def run(cfg, inp):
    nc = build(cfg)
    maps = make_in_maps(cfg, inp)
    res = run_bass_kernel_spmd(nc, maps, core_ids=list(range(8)))
    return res.results


def kernel(**inputs):
    cfg = Cfg()
    res = run(cfg, inputs)
    ys = np.zeros((2, cfg.S[0], D), np.float32)
    yp = np.zeros((2, cfg.S[1], D), np.float32)
    for c in range(8):
        b, q = c // 4, c % 4
        y = np.asarray(res[c]["y"])
        ys[b, q * cfg.T[0]:(q + 1) * cfg.T[0]] = y[:cfg.T[0]]
        yp[b, q * cfg.T[1]:(q + 1) * cfg.T[1]] = y[cfg.T[0]:]
    return (yp, ys)
```

```python
from contextlib import ExitStack
import numpy as np
import concourse.bass as bass
import concourse.mybir as mybir
from concourse.bass_utils import run_bass_kernel_spmd

F32 = mybir.dt.float32
BF16 = mybir.dt.bfloat16
I32 = mybir.dt.int32
ALU = mybir.AluOpType
AF = mybir.ActivationFunctionType
AX = mybir.AxisListType

D = 1024
KC = 8
HD = 64
H = 1024
NEG = -30000.0
RMS_EPS = 1e-6
DIL = (1, 4, 16)
TOPK = 4
BS = 512

DEBUG = {}


class Cfg:
    def __init__(self, S0=16384, S1=8192, NE=32, FF=1024, stop_after=None):
        self.S = (S0, S1)
        self.T = (S0 // 4, S1 // 4)
        self.E = (self.T[0] + 2 * H, self.T[1] + 2 * H)
        self.NE = NE
        self.FF = FF
        self.stop_after = stop_after
        assert self.T[0] % 2048 == 0 and self.T[1] % 2048 == 0
        self.ET = self.E[0] + self.E[1]
        self.TT = self.T[0] + self.T[1]

    def nblk(self, ntok):
        return (ntok * TOPK + self.NE * (BS - 1) + BS - 1) // BS


class Buf:
    __slots__ = ("name", "w", "r", "dsem")

    def __init__(self, name):
        self.name = name
        self.w = []
        self.r = []
        self.dsem = None


class Op:
    __slots__ = ("eng", "fn", "deps", "dma", "dsem", "dval", "signal", "sig", "ident")

    def __init__(self, eng, fn):
        self.eng = eng
        self.fn = fn
        self.deps = []
        self.dma = False
        self.dsem = None
        self.dval = 0
        self.signal = False
        self.sig = 0


ENGS = ("sp", "act", "pool", "dve", "pe")
N_DSEM = 56


class Sched:
    def __init__(self):
        self.ops = {e: [] for e in ENGS}
        self.dsem_count = [0] * N_DSEM
        self.dsem_next = 0
        self.phase_bufs = []
        self.dma_since_barrier = {}

    def buf(self, name):
        b = Buf(name)
        self.phase_bufs.append(b)
        return b

    def _dsem_for(self, b):
        if b.dsem is None:
            assert self.dsem_next < N_DSEM, "out of dma sems in this phase"
            b.dsem = self.dsem_next
            self.dsem_next += 1
        return b.dsem

    @staticmethod
    def _merge(lst, ev):
        if ev[0] == "dma":
            for i, o in enumerate(lst):
                if o[0] == "dma" and o[1] == ev[1]:
                    if o[2] < ev[2]:
                        lst[i] = ev
                    return
        else:
            for i, o in enumerate(lst):
                if o[0] == "op" and o[1].eng == ev[1].eng:
                    if o[1].ident < ev[1].ident:
                        lst[i] = ev
                    return
        lst.append(ev)

    def op(self, eng, fn, reads=(), writes=(), dma_buf=None):
        o = Op(eng, fn)
        o.ident = len(self.ops[eng])
        deps = []
        for b in reads:
            for ev in b.w:
                self._merge(deps, ev)
        for b in writes:
            for ev in b.w:
                self._merge(deps, ev)
            for ev in b.r:
                self._merge(deps, ev)
        if dma_buf is not None:
            o.dma = True
            o.dsem = self._dsem_for(dma_buf)
            self.dsem_count[o.dsem] += 16
            o.dval = self.dsem_count[o.dsem]
            ev = ("dma", o.dsem, o.dval)
            self.dma_since_barrier[o.dsem] = o.dval
        else:
            ev = ("op", o)
        for d in deps:
            if d[0] == "op":
                if d[1].eng == "pe" and eng == "pe":
                    continue
                d[1].signal = True
            o.deps.append(d)
        for b in writes:
            b.w = [ev]
            b.r = []
        for b in reads:
            if b in writes:
                continue
            self._merge(b.r, ev)
        self.ops[eng].append(o)
        return o

    def barrier(self):
        o = Op("sp", lambda e: e.nop())
        o.ident = len(self.ops["sp"])
        for e in ENGS:
            if e == "sp":
                continue
            for cand in reversed(self.ops[e]):
                if not cand.dma and cand.fn is not None:
                    cand.signal = True
                    o.deps.append(("op", cand))
                    break
        for s_, v in self.dma_since_barrier.items():
            o.deps.append(("dma", s_, v))
        self.dma_since_barrier = {}
        o.signal = True
        self.ops["sp"].append(o)
        for e in ENGS:
            if e == "sp":
                continue
            w = Op(e, None)
            w.ident = len(self.ops[e])
            w.deps.append(("op", o))
            self.ops[e].append(w)
        for b in self.phase_bufs:
            b.w = []
            b.r = []
            b.dsem = None
        self.phase_bufs = [b for b in self.phase_bufs if b.name.startswith("dram:")]
        self.dsem_next = 0
        return o

    def emit(self, nc, stack):
        esem = {e: stack.enter_context(nc.semaphore("es_" + e)) for e in ENGS}
        dsem = [stack.enter_context(nc.semaphore("ds%d" % i)) for i in range(N_DSEM)]
        for e in ENGS:
            c = 0
            for o in self.ops[e]:
                if o.signal and not o.dma:
                    c += 1
                    o.sig = c
        if DEBUG.get("verbose"):
            print("signals", {e: max([o.sig for o in self.ops[e]] + [0]) for e in ENGS}, "dma sem max", max(self.dsem_count),
                  "ops", {e: len(self.ops[e]) for e in ENGS})
        block = stack.enter_context(nc.Block())
        handles = {"sp": block.sync, "act": block.scalar, "pool": block.gpsimd,
                   "dve": block.vector, "pe": block.tensor}

        def make(e):
            def body(eng):
                waited = {}
                for o in self.ops[e]:
                    for d in o.deps:
                        if d[0] == "op":
                            key, val = ("e", d[1].eng), d[1].sig
                            sem = esem[d[1].eng]
                            assert val > 0, (e, d[1].eng, d[1].ident, d[1].fn, o.ident)
                        else:
                            key, val = ("d", d[1]), d[2]
                            sem = dsem[d[1]]
                        if waited.get(key, 0) >= val:
                            continue
                        waited[key] = val
                        eng.wait_ge(sem, val)
                    if o.fn is None:
                        continue
                    ins = o.fn(eng)
                    if o.dma:
                        ins.then_inc(dsem[o.dsem], 16)
                    elif o.signal:
                        ins.then_inc(esem[e], 1)
            return body

        for e in ENGS:
            handles[e](make(e))


class Builder:
    def __init__(self, cfg):
        self.cfg = cfg
        self.nc = bass.Bass("TRN2", target_bir_lowering=False)
        self.s = Sched()
        self.stack = ExitStack()
        self.dram = {}
        self.dbuf = {}
        self.arena_off = 0
        self.ARENA = 196 * 1024

    def din(self, name, shape, dt=F32):
        if name in DEBUG.get("skip_inputs", ()):
            t = self.nc.dram_tensor(name, list(shape), dt, kind="Internal")
            self.dram[name] = t
            return t
        t = self.nc.dram_tensor(name, list(shape), dt, kind="ExternalInput")
        self.dram[name] = t
        return t

    def dout(self, name, shape, dt=F32):
        t = self.nc.dram_tensor(name, list(shape), dt, kind="ExternalOutput")
        self.dram[name] = t
        self.dbuf[name] = self.s.buf("dram:" + name)
        return t

    def dscr(self, name, shape, dt=F32):
        kind = "ExternalOutput" if name in DEBUG.get("dump", ()) else "Internal"
        t = self.nc.dram_tensor(name, list(shape), dt, kind=kind)
        self.dram[name] = t
        self.dbuf[name] = self.s.buf("dram:" + name)
        return t

    def reset_arena(self, keep=0):
        self.arena_off = keep

    def sb(self, name, shape, dt, parts=128):
        esz = 2 if dt == BF16 else 4
        n = int(np.prod(shape))
        nbytes = (n * esz + 31) // 32 * 32
        off = self.arena_off
        self.arena_off += nbytes
        assert self.arena_off <= self.ARENA, (name, self.arena_off)
        ap = self.arena[:, off // 2: off // 2 + (n * esz) // 2]
        if esz == 4:
            ap = ap.bitcast(dt)
        if len(shape) == 2:
            ap = ap.rearrange("p (a b) -> p a b", a=shape[0])
        elif len(shape) == 3:
            ap = ap.rearrange("p (a b c) -> p a b c", a=shape[0], b=shape[1])
        elif len(shape) == 4:
            ap = ap.rearrange("p (a b c d) -> p a b c d", a=shape[0], b=shape[1], c=shape[2])
        return ap, self.s.buf(name)

    def pool(self, name, shape, dt, n):
        return [self.sb("%s%d" % (name, i), shape, dt) for i in range(n)]


def build(cfg):
    B = Builder(cfg)
    nc, s, st = B.nc, B.s, B.stack
    NE, FF = cfg.NE, cfg.FF
    S, T, E = cfg.S, cfg.T, cfg.E
    SP0 = S[0] + 2 * H

    xs = [B.din("xs0", [S[0], D]), B.din("xs1", [S[1], D])]
    xw = B.din("xw", [cfg.ET, D])
    cvec = B.din("cvec", [2, D])
    norm_mix_g = B.din("norm_mix_g", [2, D]); norm_ffn_g = B.din("norm_ffn_g", [2, D])
    w_ada = B.din("w_ada", [2, D, 6 * D]); b_ada = B.din("b_ada", [2, 6 * D])
    w_qkv_a = B.din("w_qkv_a", [D, 1536]); q_norm_a = B.din("q_norm_a", [1, 64]); k_norm_a = B.din("k_norm_a", [1, 64])
    w_o_a = B.din("w_o_a", [D, D])
    w_qkv_b = B.din("w_qkv_b", [D, 9216]); q_norm_b = B.din("q_norm_b", [3, 64]); k_norm_b = B.din("k_norm_b", [3, 64])
    w_o_b = B.din("w_o_b", [D, D]); rel_bias = B.din("rel_bias", [32, 48])
    w_router = B.din("w_router", [2, D, NE]); b_router = B.din("b_router", [2, NE])
    w_gu = B.din("w_gate_up", [2 * NE, D, 2 * FF]); b_gu = B.din("b_gate_up", [2 * NE * (2 * FF // 128), 128])
    w_dn = B.din("w_down", [2 * NE, FF, D]); b_dn = B.din("b_down", [2 * NE, D])
    c_ident = B.din("c_ident", [128, 128]); c_anti = B.din("c_anti", [128, 128])
    c_ustrict = B.din("c_ustrict", [128, 128])
    c_une = B.din("c_une", [2, NE, NE])
    NBMAX = cfg.nblk(cfg.ET)
    c_iotab = B.din("c_iotab", [NE, NBMAX])
    ropek = B.din("ropek", [S[0], 128])
    ropew = B.din("ropew", [cfg.ET, 128])
    c_piota = B.din("c_piota", [128, 1])
    NKM = sum(DIL[g] * (T[sg] // (128 * DIL[g]) + 1) for g in range(3) for sg in range(2))
    kmaskT = B.din("kmaskT", [128, NKM])
    c_oh = B.din("c_oh", [3, 2, 33, 256])

    yout = B.dout("y", [cfg.TT, D])

    modv = B.dscr("modv", [2, 2, 6, D])
    kT0 = [B.dscr("kT0_%d" % i, [128, 2, S[i]], BF16) for i in range(2)]
    vx0 = [B.dscr("vx0_%d" % i, [S[i], 4, 65], BF16) for i in range(2)]
    qT0 = [B.dscr("qT0_%d" % i, [128, 2, 4, E[i]], BF16) for i in range(2)]
    x1 = B.dscr("x1", [cfg.ET, D])
    x2 = B.dscr("x2", [cfg.ET, D])
    x3 = B.dscr("x3", [cfg.TT, D])
    hfb = B.dscr("hfb", [cfg.ET, D], BF16)
    NR0 = NBMAX * BS
    xsr = B.dscr("xsr", [NR0, D], BF16)
    ysr = B.dscr("ysr", [NR0, D])
    NFC = FF // 128
    wgb = B.dscr("wgb", [2 * NE * 128, 8 * 2 * FF], BF16)
    wdb = B.dscr("wdb", [2 * NE * 128, NFC * D], BF16)
    fvs = B.dscr("fvs", [3, 2, 16, 256])
    NI = [[T[sg] // DIL[g] + 128 for sg in range(2)] for g in range(3)]
    kT1 = [[B.dscr("kT1_%d_%d" % (g, sg), [128, 8, DIL[g] * NI[g][sg]], BF16) for sg in range(2)] for g in range(3)]
    qT1 = [[B.dscr("qT1_%d_%d" % (g, sg), [128, 8, DIL[g] * NI[g][sg]], BF16) for sg in range(2)] for g in range(3)]
    vx1 = [[B.dscr("vx1_%d_%d" % (g, sg), [DIL[g] * NI[g][sg], 16, 65], BF16) for sg in range(2)] for g in range(3)]
    num1 = [B.dscr("num1_%d" % g, [cfg.TT, 16, 65]) for g in range(3)]

    def db(name):
        return B.dbuf[name]

    _bc = {}

    def bchk(e, n):
        if n not in _bc:
            r = e.alloc_register("bc%d" % len(_bc))
            e.reg_mov(r, n)
            _bc[n] = e.snap(r, min_val=n, max_val=n)
        return _bc[n]

    B.arena = st.enter_context(nc.sbuf_tensor("arena", [128, B.ARENA // 2], BF16))
    psum = st.enter_context(nc.psum_tensor("ps", [128, 8, 512], F32))
    PS = [(psum[:, i, :], s.buf("ps%d" % i)) for i in range(8)]

    def psb(i):
        return psum[:, i, :].bitcast(BF16)

    id32, id32B = B.sb("id32", [128], F32)
    idb, idbB = B.sb("idb", [128], BF16)
    antib, antibB = B.sb("antib", [128], BF16)
    ones_b, onesB = B.sb("ones_b", [512], BF16)
    ones_f, onesfB = B.sb("ones_f", [128], F32)
    tmpc, tmpcB = B.sb("tmpc", [128], F32)
    s.op("sp", lambda e: e.dma_start(out=id32, in_=c_ident.ap()), writes=[id32B], dma_buf=id32B)
    s.op("sp", lambda e: e.dma_start(out=tmpc, in_=c_anti.ap()), writes=[tmpcB], dma_buf=tmpcB)
    s.op("dve", lambda e: e.tensor_copy(out=idb, in_=id32), reads=[id32B], writes=[idbB])
    s.op("dve", lambda e: e.tensor_copy(out=antib, in_=tmpc), reads=[tmpcB], writes=[antibB])
    s.op("dve", lambda e: e.memset(ones_b, 1.0), writes=[onesB])
    s.op("dve", lambda e: e.memset(ones_f, 1.0), writes=[onesfB])
    KEEP = B.arena_off

    def load_bc(eng_name, dst, dstB, src_row_ap, nparts=128):
        s.op(eng_name, lambda e: e.dma_start(out=dst, in_=src_row_ap.partition_broadcast(nparts)),
             writes=[dstB], dma_buf=dstB)

    def rstd(ss, ssB, n_inv):
        s.op("act", lambda e: e.activation(out=ss, in_=ss, func=AF.Ln, scale=n_inv, bias=epsc[:, 0:1]),
             reads=[ssB, epscB], writes=[ssB])
        s.op("act", lambda e: e.activation(out=ss, in_=ss, func=AF.Exp, scale=-0.5), reads=[ssB], writes=[ssB])

    def norm_tile(x, xB, Abc, Bbc, bcB, sq, sqB, ss, ssB, t32, t32B, out, outB):
        s.op("act", lambda e: e.activation(out=sq, in_=x, func=AF.Square), reads=[xB], writes=[sqB])
        s.op("dve", lambda e: e.reduce_sum(out=ss, in_=sq, axis=AX.X), reads=[sqB], writes=[ssB])
        rstd(ss, ssB, 1.0 / D)
        s.op("dve", lambda e: e.scalar_tensor_tensor(out=t32, in0=x, scalar=ss[:, 0:1], in1=Abc, op0=ALU.mult, op1=ALU.mult),
             reads=[xB, ssB, bcB], writes=[t32B])
        s.op("dve", lambda e: e.tensor_tensor(out=out, in0=t32, in1=Bbc, op=ALU.add), reads=[t32B, bcB], writes=[outB])

    def transpose8(src, srcB, ident, identB, bank, dst, dstB, eng="act"):
        pv = psb(bank)

        def f(e):
            for k in range(8):
                ins = e.transpose(out=pv[:, k * 128:(k + 1) * 128], in_=src[:, k * 128:(k + 1) * 128], identity=ident)
            return ins
        s.op("pe", f, reads=[srcB, identB], writes=[PS[bank][1]])
        if eng == "act":
            s.op("act", lambda e: e.copy(out=dst.rearrange("p a b -> p (a b)"), in_=pv), reads=[PS[bank][1]], writes=[dstB])
        else:
            s.op("dve", lambda e: e.tensor_copy(out=dst.rearrange("p a b -> p (a b)"), in_=pv), reads=[PS[bank][1]], writes=[dstB])

    def proj(hmT, hmTB, W, WB, c0, ncols, bank):
        def f(e):
            for k in range(8):
                ins = e.matmul(PS[bank][0][:, 0:ncols], lhsT=hmT[:, k, :], rhs=W[:, k, c0:c0 + ncols], start=(k == 0), stop=(k == 7))
            return ins
        s.op("pe", f, reads=[hmTB, WB], writes=[PS[bank][1]])

    def headnorm(src, srcB, nh, gbc, gbcB, sq, sqB, ssh, sshB, out, outB):
        s.op("act", lambda e: e.activation(out=sq[:, 0:nh * 64], in_=src, func=AF.Square), reads=[srcB], writes=[sqB])
        s.op("dve", lambda e: e.reduce_sum(out=ssh[:, 0:nh], in_=sq[:, 0:nh * 64].rearrange("p (h c) -> p h c", c=64), axis=AX.X),
             reads=[sqB], writes=[sshB])
        s.op("act", lambda e: e.activation(out=ssh[:, 0:nh], in_=ssh[:, 0:nh], func=AF.Ln, scale=1.0 / 64, bias=epsc[:, 0:1]),
             reads=[sshB, epscB], writes=[sshB])
        s.op("act", lambda e: e.activation(out=ssh[:, 0:nh], in_=ssh[:, 0:nh], func=AF.Exp, scale=-0.5), reads=[sshB], writes=[sshB])
        s3 = src.rearrange("p (h c) -> p h c", c=64)
        o3 = out.rearrange("p (h c) -> p h c", c=64)
        s.op("dve", lambda e: e.tensor_tensor(out=o3, in0=s3, in1=ssh[:, 0:nh].unsqueeze(2).to_broadcast([128, nh, 64]), op=ALU.mult),
             reads=[srcB, sshB], writes=[outB])
        s.op("dve", lambda e: e.tensor_tensor(out=o3, in0=o3, in1=gbc.unsqueeze(1).to_broadcast([128, nh, 64]), op=ALU.mult),
             reads=[outB, gbcB], writes=[outB])

    def rope(src, srcB, nh, cs, csB, sw, swB, t1, t1B, out, outB):
        s5 = src.rearrange("p (h a w c) -> p h a w c", a=2, w=2, c=16)
        w5 = sw[:, 0:nh * 64].rearrange("p (h a w c) -> p h a w c", a=2, w=2, c=16)
        s.op("act", lambda e: e.copy(out=w5[:, :, :, 0, :], in_=s5[:, :, :, 1, :]), reads=[srcB], writes=[swB])
        s.op("act", lambda e: e.copy(out=w5[:, :, :, 1, :], in_=s5[:, :, :, 0, :]), reads=[srcB, swB], writes=[swB])
        cosb = cs[:, 0:64].unsqueeze(1).to_broadcast([128, nh, 64])
        sinb = cs[:, 64:128].unsqueeze(1).to_broadcast([128, nh, 64])
        s3 = src.rearrange("p (h c) -> p h c", c=64)
        w3 = sw[:, 0:nh * 64].rearrange("p (h c) -> p h c", c=64)
        a3 = t1[:, 0:nh * 64].rearrange("p (h c) -> p h c", c=64)
        o3 = out.rearrange("p (h c) -> p h c", c=64)
        s.op("dve", lambda e: e.tensor_tensor(out=a3, in0=s3, in1=cosb, op=ALU.mult), reads=[srcB, csB], writes=[t1B])
        s.op("dve", lambda e: e.tensor_tensor(out=w3, in0=w3, in1=sinb, op=ALU.mult), reads=[swB, csB], writes=[swB])
        s.op("dve", lambda e: e.tensor_tensor(out=o3, in0=a3, in1=w3, op=ALU.add), reads=[t1B, swB], writes=[outB])

    def load_w_bf16(dst, dstB, src2d, c0, ncols):
        s.op("pool", lambda e: e.dma_start(out=dst, in_=src2d[:, c0:c0 + ncols].rearrange("(k p) c -> p k c", p=128)),
             writes=[dstB], dma_buf=dstB)

    B.reset_arena(KEEP)
    epsc, epscB = B.sb("epsc", [1], F32)
    s.op("dve", lambda e: e.memset(epsc, RMS_EPS), writes=[epscB])
    KEEP = B.arena_off
    cT, cTB = B.sb("cT", [8, 2], F32)
    mrow, mrowB = B.sb("mrow", [6 * D], F32)
    brow, browB = B.sb("brow", [6 * D], F32)
    grow, growB = B.sb("grow", [2, D], F32)
    wa = B.pool("wa", [8, 512], F32, 2)
    for g_ in range(2):
        def f_ct(e, g_=g_):
            with nc.allow_non_contiguous_dma(reason="tiny transposed load of c"):
                return e.dma_start(out=cT[:, :, g_], in_=cvec.ap()[g_].rearrange("(k p) -> p k", p=128))
        s.op("sp", f_ct, writes=[cTB], dma_buf=cTB)
    s.op("act", lambda e: e.activation(out=cT, in_=cT, func=AF.Silu), reads=[cTB], writes=[cTB])
    for l in range(2):
        load_bc("sp", brow[0:2, :], browB, b_ada.ap()[l:l + 1, :], 2)
        load_bc("sp", grow[0:2, 0, :], growB, norm_mix_g.ap()[l:l + 1, :], 2)
        load_bc("sp", grow[0:2, 1, :], growB, norm_ffn_g.ap()[l:l + 1, :], 2)
        for cc in range(12):
            wt, wtB = wa[cc % 2]
            s.op("sp", lambda e, wt=wt, cc=cc, l=l: e.dma_start(out=wt, in_=w_ada.ap()[l, :, cc * 512:(cc + 1) * 512].rearrange("(k p) c -> p k c", p=128)),
                 writes=[wtB], dma_buf=wtB)
            bank = cc % 2

            def f(e, wt=wt, bank=bank):
                for k in range(8):
                    ins = e.matmul(PS[bank][0][0:2, :], lhsT=cT[:, k, :], rhs=wt[:, k, :], start=(k == 0), stop=(k == 7))
                return ins
            s.op("pe", f, reads=[cTB, wtB], writes=[PS[bank][1]])
            s.op("dve", lambda e, bank=bank, cc=cc: e.tensor_tensor(out=mrow[0:2, cc * 512:(cc + 1) * 512], in0=PS[bank][0][0:2, :],
                                                                  in1=brow[0:2, cc * 512:(cc + 1) * 512], op=ALU.add),
                 reads=[PS[bank][1], browB], writes=[mrowB])
        for j, gi in ((1, 0), (4, 1)):
            s.op("dve", lambda e, j=j, gi=gi: e.scalar_tensor_tensor(out=mrow[0:2, j * D:(j + 1) * D], in0=mrow[0:2, j * D:(j + 1) * D], scalar=1.0,
                                                                  in1=grow[0:2, gi, :], op0=ALU.add, op1=ALU.mult),
                 reads=[mrowB, growB], writes=[mrowB])
        s.op("sp", lambda e, l=l: e.dma_start(out=modv.ap()[l].rearrange("g s d -> g (s d)"), in_=mrow[0:2, :]),
             reads=[mrowB], writes=[db("modv")], dma_buf=mrowB)
    s.barrier()
    if cfg.stop_after == "P0":
        return finish(B, cfg)

    B.reset_arena(KEEP)
    Wa, WaB = B.sb("Wa", [8, 1536], BF16)
    load_w_bf16(Wa, WaB, w_qkv_a.ap(), 0, 1536)
    gq, gqB = B.sb("gq", [64], F32); gk, gkB = B.sb("gk", [64], F32)
    load_bc("sp", gq, gqB, q_norm_a.ap()[0:1, :]); load_bc("sp", gk, gkB, k_norm_a.ap()[0:1, :])
    bcm = [B.sb("bcm%d" % i, [2, D], F32) for i in range(2)]
    for sg in range(2):
        load_bc("sp", bcm[sg][0][:, 0, :], bcm[sg][1], modv.ap()[0, sg, 1:2, :].rearrange("a d -> a d"))
        load_bc("sp", bcm[sg][0][:, 1, :], bcm[sg][1], modv.ap()[0, sg, 0:1, :])
    xt = B.pool("xt", [D], F32, 2)
    cst = B.pool("cst", [128], F32, 2)
    sq, sqB = B.sb("sq", [D], F32)
    ss, ssB = B.sb("ss", [1], F32)
    t32, t32B = B.sb("t32", [D], F32)
    hm = B.pool("hm", [D], BF16, 2)
    hmT = B.pool("hmT", [8, 128], BF16, 2)
    qf, qfB = B.sb("qf", [D], F32)
    qn, qnB = B.sb("qn", [D], F32)
    sw, swB = B.sb("sw", [D], F32)
    t1, t1B = B.sb("t1", [D], F32)
    ssh, sshB = B.sb("ssh", [16], F32)
    kb = B.pool("kb", [256], BF16, 2)
    qb = B.pool("qb", [D], BF16, 2)
    vxt = B.pool("vxt", [4, 65], BF16, 2)
    for i in range(2):
        s.op("dve", lambda e, i=i: e.memset(vxt[i][0], 1.0), writes=[vxt[i][1]])
    kTt = B.pool("kTt", [2, 128], BF16, 2)
    qTt = B.pool("qTt", [8, 128], BF16, 2)
    qbn, qbnB = B.sb("qbn", [D], BF16)
    segoff = (0, E[0])
    it = 0
    for sg in range(2):
        ntk = S[sg] // 128
        nte = E[sg] // 128
        for j in range(ntk + nte):
            isq = j >= ntk
            jj = j - ntk if isq else j
            x, xB = xt[it % 2]; cs, csB = cst[it % 2]
            hmb, hmbB = hm[it % 2]; hT, hTB = hmT[it % 2]
            if not isq:
                r0 = jj * 128
                s.op("sp", lambda e, x=x, r0=r0, sg=sg: e.dma_start(out=x, in_=xs[sg].ap()[r0:r0 + 128, :]), writes=[xB], dma_buf=xB)
                s.op("sp", lambda e, cs=cs, r0=r0: e.dma_start(out=cs, in_=ropek.ap()[r0:r0 + 128, :]), writes=[csB], dma_buf=csB)
            else:
                r0 = segoff[sg] + jj * 128
                s.op("sp", lambda e, x=x, r0=r0: e.dma_start(out=x, in_=xw.ap()[r0:r0 + 128, :]), writes=[xB], dma_buf=xB)
                s.op("sp", lambda e, cs=cs, r0=r0: e.dma_start(out=cs, in_=ropew.ap()[r0:r0 + 128, :]), writes=[csB], dma_buf=csB)
            A_, Bc_ = bcm[sg][0][:, 0, :], bcm[sg][0][:, 1, :]
            norm_tile(x, xB, A_, Bc_, bcm[sg][1], sq, sqB, ss, ssB, t32, t32B, hmb, hmbB)
            transpose8(hmb, hmbB, idb, idbB, 0 + (it % 2), hT, hTB)
            if not isq:
                proj(hT, hTB, Wa, WaB, 1024, 512, 2 + (it % 2))
                pk = PS[2 + (it % 2)]
                s.op("act", lambda e, pk=pk: e.copy(out=qf[:, 0:512], in_=pk[0]), reads=[pk[1]], writes=[qfB])
                headnorm(qf[:, 0:256], qfB, 4, gk, gkB, sq, sqB, ssh, sshB, qn[:, 0:256], qnB)
                kbt, kbB = kb[it % 2]
                rope(qn[:, 0:256], qnB, 4, cs, csB, sw, swB, t1, t1B, kbt, kbB)
                vt, vtB = vxt[it % 2]
                s.op("dve", lambda e, vt=vt: e.tensor_copy(out=vt[:, :, 0:64], in_=qf[:, 256:512].rearrange("p (h c) -> p h c", c=64)),
                     reads=[qfB], writes=[vtB])
                s.op("sp", lambda e, vt=vt, jj=jj, sg=sg: e.dma_start(out=vx0[sg].ap()[jj * 128:(jj + 1) * 128, :, :], in_=vt),
                     reads=[vtB], writes=[db("vx0_%d" % sg)], dma_buf=vtB)
                bank = 4 + (it % 2)
                pv = psb(bank)

                def ft(e, kbt=kbt, pv=pv):
                    for p in range(2):
                        ins = e.transpose(out=pv[:, p * 128:(p + 1) * 128], in_=kbt[:, p * 128:(p + 1) * 128], identity=idb)
                    return ins
                s.op("pe", ft, reads=[kbB, idbB], writes=[PS[bank][1]])
                kt, ktB = kTt[it % 2]
                s.op("act", lambda e, kt=kt, pv=pv: e.copy(out=kt.rearrange("p a b -> p (a b)"), in_=pv[:, 0:256]), reads=[PS[bank][1]], writes=[ktB])
                s.op("sp", lambda e, kt=kt, jj=jj, sg=sg: e.dma_start(out=kT0[sg].ap()[:, :, jj * 128:(jj + 1) * 128], in_=kt),
                     reads=[ktB], writes=[db("kT0_%d" % sg)], dma_buf=ktB)
            else:
                proj(hT, hTB, Wa, WaB, 0, 512, 2)
                proj(hT, hTB, Wa, WaB, 512, 512, 3)
                s.op("act", lambda e: e.copy(out=qf[:, 0:512], in_=PS[2][0]), reads=[PS[2][1]], writes=[qfB])
                s.op("act", lambda e: e.copy(out=qf[:, 512:1024], in_=PS[3][0]), reads=[PS[3][1], qfB], writes=[qfB])
                headnorm(qf, qfB, 16, gq, gqB, sq, sqB, ssh, sshB, qn, qnB)
                qbt, qbB = qb[it % 2]
                rope(qn, qnB, 16, cs, csB, sw, swB, t1, t1B, qbn, qbnB)
                srcv = qbn.rearrange("q (p u i c) -> q p u i c", p=2, u=2, i=4)
                dstv = qbt.rearrange("q (p i u c) -> q p i u c", p=2, i=4, u=2)
                for u_ in range(2):
                    s.op("act", lambda e, u_=u_, srcv=srcv, dstv=dstv: e.copy(out=dstv[:, :, :, u_, :], in_=srcv[:, :, u_, :, :]),
                         reads=[qbnB, qbB], writes=[qbB])
                bank = 4 + (it % 2)
                pv = psb(bank)

                def ftq(e, qbt=qbt, pv=pv):
                    for pi in range(8):
                        ins = e.transpose(out=pv[:, pi * 128:(pi + 1) * 128], in_=qbt[:, pi * 128:(pi + 1) * 128], identity=idb)
                    return ins
                s.op("pe", ftq, reads=[qbB, idbB], writes=[PS[bank][1]])
                qt, qtB = qTt[it % 2]
                s.op("act", lambda e, qt=qt, pv=pv: e.copy(out=qt.rearrange("p a b -> p (a b)"), in_=pv), reads=[PS[bank][1]], writes=[qtB])
                s.op("sp", lambda e, qt=qt, jj=jj, sg=sg: e.dma_start(
                    out=qT0[sg].ap().rearrange("p a i n -> p (a i) n")[:, :, jj * 128:(jj + 1) * 128], in_=qt),
                    reads=[qtB], writes=[db("qT0_%d" % sg)], dma_buf=qtB)
            it += 1
    s.barrier()
    if cfg.stop_after == "P1":
        return finish(B, cfg)

    oT0 = [B.dscr("oT0_%d" % i, [64, 16, E[i]], BF16) for i in range(2)]

    def attn_seg(sg):
        B.reset_arena(KEEP)
        Sn = S[sg]
        nkt = Sn // 128
        KT, KTB = B.sb("KT", [Sn], BF16)
        VX, VXB = B.sb("VX", [nkt, 2, 65], BF16)
        QTp = B.pool("QTp", [4, 128], BF16, 2)
        pTp = B.pool("pTp", [2, 512], BF16, 3)
        o32, o32B = B.sb("o32", [512], F32)
        rc, rcB = B.sb("rc", [512], F32)
        oTn = B.pool("oTn", [512], BF16, 2)
        cnt = 0
        for p in range(2):
            s.op("sp", lambda e, p=p, sg=sg: e.dma_start(out=KT, in_=kT0[sg].ap()[:, p, :]), reads=[db("kT0_%d" % sg)], writes=[KTB], dma_buf=KTB)
            for t0 in range(0, nkt, 16):
                s.op("sp", lambda e, p=p, sg=sg, t0=t0: e.dma_start(
                    out=VX[:, t0:t0 + 16, :, :], in_=vx0[sg].ap()[t0 * 128:(t0 + 16) * 128, 2 * p:2 * p + 2, :].rearrange("(t k) u c -> k t u c", k=128)),
                    reads=[db("vx0_%d" % sg)], writes=[VXB], dma_buf=VXB)
            for jq in range(E[sg] // 128):
                QT, QTB = QTp[jq % 2]
                s.op("sp", lambda e, QT=QT, p=p, jq=jq, sg=sg: e.dma_start(out=QT, in_=qT0[sg].ap()[:, p, :, jq * 128:(jq + 1) * 128]),
                     reads=[db("qT0_%d" % sg)], writes=[QTB], dma_buf=QTB)
                for u in range(2):
                    acc = 4 + (cnt % 2)
                    cnt += 1
                    lo, hi = 64 * u, 64 * u + 64
                    nst = nkt // 2
                    def rec_scores(st_, lo=lo, hi=hi, QT=QT, QTB=QTB):
                        sb0 = 2 * (st_ % 2)

                        def fs(e, st_=st_, sb0=sb0):
                            for t_ in range(2):
                                k0 = (2 * st_ + t_) * 128
                                ins = e.matmul(PS[sb0 + t_][0], lhsT=KT[lo:hi, k0:k0 + 128],
                                               rhs=QT[lo:hi, :, :].rearrange("p a b -> p (a b)"), start=True, stop=True)
                            return ins
                        s.op("pe", fs, reads=[KTB, QTB], writes=[PS[sb0][1], PS[sb0 + 1][1]])

                    rec_scores(0)
                    for st_ in range(nst):
                        sb0 = 2 * (st_ % 2)
                        pT, pTB = pTp[st_ % 3]
                        s.op("act", lambda e, sb0=sb0, pT=pT: e.activation(out=pT.rearrange("p a b -> p (a b)"),
                                                                        in_=psum[:, sb0:sb0 + 2, :].rearrange("p a b -> p (a b)"),
                                                                        func=AF.Exp, scale=0.125),
                             reads=[PS[sb0][1], PS[sb0 + 1][1]], writes=[pTB])
                        if st_ + 1 < nst:
                            rec_scores(st_ + 1)

                        def fv(e, st_=st_, pT=pT, u=u, acc=acc, nst=nst):
                            for t_ in range(2):
                                kt_ = 2 * st_ + t_
                                ins = e.matmul(PS[acc][0][0:65, :], lhsT=VX[:, kt_, u, :], rhs=pT[:, t_, :],
                                               start=(kt_ == 0), stop=(kt_ == 2 * nst - 1))
                            return ins
                        s.op("pe", fv, reads=[VXB, pTB], writes=[PS[acc][1]])
                    s.op("act", lambda e, acc=acc: e.copy(out=o32[0:65, :], in_=PS[acc][0][0:65, :]), reads=[PS[acc][1]], writes=[o32B])
                    s.op("pe", lambda e: e.matmul(PS[6][0][0:64, :], lhsT=ones_f[64:65, 0:64], rhs=o32[64:65, :], start=True, stop=True),
                         reads=[o32B, onesfB], writes=[PS[6][1]])
                    s.op("dve", lambda e: e.reciprocal(out=rc[0:64, :], in_=PS[6][0][0:64, :]), reads=[PS[6][1]], writes=[rcB])
                    on, onB = oTn[cnt % 2]
                    s.op("dve", lambda e, on=on: e.tensor_tensor(out=on[0:64, :], in0=o32[0:64, :], in1=rc[0:64, :], op=ALU.mult),
                         reads=[o32B, rcB], writes=[onB])
                    h = 2 * p + u
                    s.op("sp", lambda e, on=on, h=h, jq=jq, sg=sg: e.dma_start(
                        out=oT0[sg].ap()[:, 4 * h:4 * h + 4, jq * 128:(jq + 1) * 128], in_=on[0:64, :].rearrange("p (a b) -> p a b", a=4)),
                        reads=[onB], writes=[db("oT0_%d" % sg)], dma_buf=onB)
        s.barrier()
    for sg_i in range(2):
        attn_seg(sg_i)
    if cfg.stop_after == "P2b1":
        return finish(B, cfg)

    def wo_phase(l, oT_fn, oTbufs, x_loader, gate_grp, ntiles_seg, dst, dstname, w_o_t):
        B.reset_arena(KEEP)
        Wo, WoB = B.sb("Wo", [16, D], BF16)
        s.op("pool", lambda e: e.dma_start(out=Wo[0:64], in_=w_o_t.ap().rearrange("(h c) n -> c h n", c=64)), writes=[WoB], dma_buf=WoB)
        gbc = [B.sb("gbc%d" % i, [D], F32) for i in range(2)]
        for g_ in range(2):
            load_bc("sp", gbc[g_][0], gbc[g_][1], modv.ap()[l, g_, 2:3, :])
        oTt = B.pool("oTt", [16, 128], BF16, 2)
        xp = B.pool("xp", [D], F32, 2)
        tmp = B.pool("tmpo", [D], F32, 2)
        row = 0
        it_ = 0
        for sg in range(2):
            for jq in range(ntiles_seg[sg]):
                oTl, oTlB = oTt[it_ % 2]
                s.op("sp", lambda e, oTl=oTl, sg=sg, jq=jq: e.dma_start(out=oTl[0:64], in_=oT_fn(sg, jq)), reads=oTbufs, writes=[oTlB], dma_buf=oTlB)
                x, xB = xp[it_ % 2]
                x_loader(x, xB, sg, jq)
                tm, tmB = tmp[it_ % 2]
                for hf in range(2):
                    bank = 2 * (it_ % 2) + hf

                    def fw(e, oTl=oTl, hf=hf, bank=bank):
                        for hd in range(16):
                            ins = e.matmul(PS[bank][0], lhsT=oTl[0:64, hd, :], rhs=Wo[0:64, hd, hf * 512:(hf + 1) * 512], start=(hd == 0), stop=(hd == 15))
                        return ins
                    s.op("pe", fw, reads=[oTlB, WoB], writes=[PS[bank][1]])
                    s.op("dve", lambda e, tm=tm, hf=hf, bank=bank, sg=sg: e.tensor_tensor(out=tm[:, hf * 512:(hf + 1) * 512], in0=PS[bank][0],
                                                                                 in1=gbc[gate_grp(sg)][0][:, hf * 512:(hf + 1) * 512], op=ALU.mult),
                         reads=[PS[bank][1], gbc[gate_grp(sg)][1]], writes=[tmB])
                s.op("dve", lambda e, tm=tm, x=x: e.tensor_tensor(out=tm, in0=tm, in1=x, op=ALU.add), reads=[tmB, xB], writes=[tmB])
                s.op("sp", lambda e, tm=tm, row=row: e.dma_start(out=dst.ap()[row:row + 128, :], in_=tm), reads=[tmB], writes=[db(dstname)], dma_buf=tmB)
                row += 128
                it_ += 1
        s.barrier()

    def x0_loader(x, xB, sg, jq):
        r0 = segoff[sg] + jq * 128
        s.op("sp", lambda e, x=x, r0=r0: e.dma_start(out=x, in_=xw.ap()[r0:r0 + 128, :]), writes=[xB], dma_buf=xB)

    wo_phase(0, lambda sg, jq: oT0[sg].ap()[:, :, jq * 128:(jq + 1) * 128], [db("oT0_0"), db("oT0_1")], x0_loader,
             lambda sg: sg, [E[0] // 128, E[1] // 128], x1, "x1", w_o_a)
    if cfg.stop_after == "P2":
        return finish(B, cfg)

    def moe(l, src, srcname, ntiles, grp_of_tile, dst, dstname):
        NT = ntiles
        NB = cfg.nblk(NT * 128)
        NR = NB * BS
        B.reset_arena(KEEP)
        G4, G4B = B.sb("G4", [NT, 4], F32)
        D4i, D4iB = B.sb("D4i", [NT, 4], I32)
        idxE, idxEB = B.sb("idxE", [NB], I32)
        idxW, idxWB = B.sb("idxW", [NB], I32)
        idx16, idx16B = B.sb("idx16", [NB], I32)
        KEEP2 = B.arena_off
        L_all, L_allB = B.sb("L_all", [NT, NE], F32)
        M_all, M_allB = B.sb("M_all", [NT, NE], F32)
        M4, M4B = B.sb("M4", [NT, 8], F32)
        D4, D4B = B.sb("D4", [NT, 4], F32)
        KEEP3 = B.arena_off
        Wr, WrB = B.sb("Wr", [8, NE], F32)
        s.op("sp", lambda e: e.dma_start(out=Wr, in_=w_router.ap()[l].rearrange("(k p) n -> p k n", p=128)), writes=[WrB], dma_buf=WrB)
        brb, brbB = B.sb("brb", [NE], F32)
        load_bc("sp", brb, brbB, b_router.ap()[l:l + 1, :])
        bcf = [B.sb("bcf%d" % i, [2, D], F32) for i in range(2)]
        for g_ in range(2):
            load_bc("sp", bcf[g_][0][:, 0, :], bcf[g_][1], modv.ap()[l, g_, 4:5, :])
            load_bc("sp", bcf[g_][0][:, 1, :], bcf[g_][1], modv.ap()[l, g_, 3:4, :])
        xp = B.pool("xm", [D], F32, 2)
        sq, sqB = B.sb("sqm", [D], F32)
        ss, ssB = B.sb("ssm", [1], F32)
        t32, t32B = B.sb("t32m", [D], F32)
        hf32 = B.pool("hf32", [D], F32, 2)
        hfbf = B.pool("hfbf", [D], BF16, 2)
        hfT = B.pool("hfT", [8, 128], F32, 2)
        e4, e4B = B.sb("e4", [4], F32)
        s4, s4B = B.sb("s4", [1], F32)
        for j in range(NT):
            x, xB = xp[j % 2]
            s.op("sp", lambda e, x=x, j=j: e.dma_start(out=x, in_=src.ap()[j * 128:(j + 1) * 128, :]), reads=[db(srcname)], writes=[xB], dma_buf=xB)
            g_ = grp_of_tile(j)
            h32, h32B = hf32[j % 2]
            norm_tile(x, xB, bcf[g_][0][:, 0, :], bcf[g_][0][:, 1, :], bcf[g_][1], sq, sqB, ss, ssB, t32, t32B, h32, h32B)
            hb, hbB = hfbf[j % 2]
            s.op("act", lambda e, hb=hb, h32=h32: e.copy(out=hb, in_=h32), reads=[h32B], writes=[hbB])
            s.op("sp", lambda e, hb=hb, j=j: e.dma_start(out=hfb.ap()[j * 128:(j + 1) * 128, :], in_=hb), reads=[hbB], writes=[db("hfb")], dma_buf=hbB)
            hT, hTB = hfT[j % 2]
            b0 = 2 * (j % 2)

            def ftr(e, h32=h32, b0=b0):
                for k in range(8):
                    ins = e.transpose(out=PS[b0 + k // 4][0][:, (k % 4) * 128:(k % 4 + 1) * 128], in_=h32[:, k * 128:(k + 1) * 128], identity=id32)
                return ins
            s.op("pe", ftr, reads=[h32B, id32B], writes=[PS[b0][1], PS[b0 + 1][1]])
            s.op("act", lambda e, hT=hT, b0=b0: e.copy(out=hT.rearrange("p a b -> p (a b)"), in_=psum[:, b0:b0 + 2, :].rearrange("p a b -> p (a b)")),
                 reads=[PS[b0][1], PS[b0 + 1][1]], writes=[hTB])
            lb = 4 + (j % 2)

            def frt(e, hT=hT, lb=lb):
                for k in range(8):
                    ins = e.matmul(PS[lb][0][:, 0:NE], lhsT=hT[:, k, :], rhs=Wr[:, k, :], start=(k == 0), stop=(k == 7))
                return ins
            s.op("pe", frt, reads=[hTB, WrB], writes=[PS[lb][1]])
            s.op("dve", lambda e, j=j, lb=lb: e.tensor_tensor(out=L_all[:, j, :], in0=PS[lb][0][:, 0:NE], in1=brb, op=ALU.add),
                 reads=[PS[lb][1], brbB], writes=[L_allB])
            s.op("dve", lambda e, j=j: e.max(out=M4[:, j, :], in_=L_all[:, j, :]), reads=[L_allB], writes=[M4B])
            s.op("dve", lambda e, j=j: e.tensor_scalar(out=M_all[:, j, :], in0=L_all[:, j, :], scalar1=M4[:, j, 3:4], scalar2=None, op0=ALU.is_ge),
                 reads=[L_allB, M4B], writes=[M_allB])
            s.op("dve", lambda e, j=j: e.tensor_scalar(out=e4, in0=M4[:, j, 0:4], scalar1=M4[:, j, 0:1], scalar2=None, op0=ALU.subtract),
                 reads=[M4B], writes=[e4B])
            s.op("act", lambda e: e.activation(out=e4, in_=e4, func=AF.Exp), reads=[e4B], writes=[e4B])
            s.op("dve", lambda e: e.reduce_sum(out=s4, in_=e4, axis=AX.X), reads=[e4B], writes=[s4B])
            s.op("dve", lambda e: e.reciprocal(out=s4, in_=s4), reads=[s4B], writes=[s4B])
            s.op("dve", lambda e, j=j: e.tensor_scalar(out=G4[:, j, :], in0=e4, scalar1=s4[:, 0:1], scalar2=None, op0=ALU.mult),
                 reads=[e4B, s4B], writes=[G4B])
        s.barrier()
        B.reset_arena(KEEP3)
        ustr, ustrB = B.sb("ustr", [128], F32)
        s.op("sp", lambda e: e.dma_start(out=ustr, in_=c_ustrict.ap()), writes=[ustrB], dma_buf=ustrB)
        une, uneB = B.sb("une", [2, NE], F32)
        s.op("sp", lambda e: e.dma_start(out=une[0:NE], in_=c_une.ap().rearrange("a e f -> e a f")), writes=[uneB], dma_buf=uneB)
        iot, iotB = B.sb("iot", [NB], F32)
        s.op("sp", lambda e: e.dma_start(out=iot[0:NE], in_=c_iotab.ap()[:, 0:NB]), writes=[iotB], dma_buf=iotB)
        cn, cnB = B.sb("cn", [4], F32)
        pbc, pbcB = B.sb("pbc", [128], F32)
        cmp_, cmpB = B.sb("cmp", [NB], F32)
        bke, bkeB = B.sb("bke", [NB], F32)
        bki, bkiB = B.sb("bki", [NB], F32)
        pio, pioB = B.sb("pio", [1], F32)
        s.op("sp", lambda e: e.dma_start(out=pio, in_=c_piota.ap()), writes=[pioB], dma_buf=pioB)
        PBR, PBRB = B.sb("PBR", [NE], F32)
        dj, djB = B.sb("dj", [NE], F32)
        tq, tqB = B.sb("tq", [NE], F32)

        def fcnt(e):
            for j in range(NT):
                ins = e.matmul(PS[0][0][0:NE, 0:1], lhsT=M_all[:, j, :], rhs=ones_f[:, 0:1], start=(j == 0), stop=(j == NT - 1))
            return ins
        s.op("pe", fcnt, reads=[M_allB, onesfB], writes=[PS[0][1]])
        s.op("dve", lambda e: e.tensor_copy(out=cn[0:NE, 0:1], in_=PS[0][0][0:NE, 0:1]), reads=[PS[0][1]], writes=[cnB])
        s.op("dve", lambda e: e.tensor_scalar(out=cmp_[0:NE, :], in0=iot[0:NE, :], scalar1=cn[0:NE, 0:1], scalar2=None, op0=ALU.is_lt),
             reads=[iotB, cnB], writes=[cmpB])
        s.op("dve", lambda e: e.reduce_sum(out=cn[0:NE, 1:2], in_=cmp_[0:NE, :], axis=AX.X), reads=[cmpB], writes=[cnB])
        s.op("dve", lambda e: e.tensor_scalar(out=cn[0:NE, 2:3], in0=cn[0:NE, 1:2], scalar1=float(BS), scalar2=None, op0=ALU.mult), reads=[cnB], writes=[cnB])
        s.op("pe", lambda e: e.matmul(PS[1][0][0:NE, 0:1], lhsT=une[0:NE, 0, :], rhs=cn[0:NE, 2:3], start=True, stop=True), reads=[uneB, cnB], writes=[PS[1][1]])
        s.op("dve", lambda e: e.tensor_copy(out=cn[0:NE, 3:4], in_=PS[1][0][0:NE, 0:1]), reads=[PS[1][1]], writes=[cnB])
        s.op("dve", lambda e: e.tensor_scalar(out=cmp_[0:NE, :], in0=iot[0:NE, :], scalar1=cn[0:NE, 3:4], scalar2=None, op0=ALU.is_ge),
             reads=[iotB, cnB], writes=[cmpB])
        s.op("pe", lambda e: e.matmul(PS[2][0][:, 0:NB], lhsT=ones_f[0:NE, :], rhs=cmp_[0:NE, :], start=True, stop=True), reads=[cmpB, onesfB], writes=[PS[2][1]])
        s.op("dve", lambda e: e.tensor_scalar(out=bke, in0=PS[2][0][:, 0:NB], scalar1=float(NE - 1), scalar2=float(l * NE), op0=ALU.min, op1=ALU.add),
             reads=[PS[2][1]], writes=[bkeB])
        s.op("dve", lambda e: e.tensor_copy(out=idxE, in_=bke), reads=[bkeB], writes=[idxEB])
        s.op("dve", lambda e: e.tensor_scalar(out=bki, in0=bke, scalar1=128.0, scalar2=pio[:, 0:1], op0=ALU.mult, op1=ALU.add), reads=[bkeB, pioB], writes=[bkiB])
        s.op("dve", lambda e: e.tensor_copy(out=idxW, in_=bki), reads=[bkiB], writes=[idxWB])
        s.op("dve", lambda e: e.tensor_scalar(out=bki, in0=bke, scalar1=float(2 * FF // 128), scalar2=pio[:, 0:1], op0=ALU.mult, op1=ALU.add), reads=[bkeB, pioB, idxWB], writes=[bkiB])
        s.op("dve", lambda e: e.tensor_copy(out=idx16, in_=bki), reads=[bkiB], writes=[idx16B])
        s.op("dve", lambda e: e.tensor_copy(out=pbc[0:NE, :], in_=cn[0:NE, 2:3].to_broadcast([NE, 128])), reads=[cnB], writes=[pbcB])
        s.op("pe", lambda e: e.matmul(PS[3][0][:, 0:NE], lhsT=pbc[0:NE, :], rhs=une[0:NE, 1, :], start=True, stop=True), reads=[pbcB, uneB], writes=[PS[3][1]])
        s.op("dve", lambda e: e.tensor_copy(out=PBR, in_=PS[3][0][:, 0:NE]), reads=[PS[3][1]], writes=[PBRB])
        for j in range(NT):
            b0 = 4 + 2 * (j % 2)
            s.op("pe", lambda e, j=j, b0=b0: e.matmul(PS[b0][0][:, 0:NE], lhsT=ustr, rhs=M_all[:, j, :], start=True, stop=True),
                 reads=[ustrB, M_allB], writes=[PS[b0][1]])
            s.op("pe", lambda e, j=j, b0=b0: e.matmul(PS[b0 + 1][0][:, 0:NE], lhsT=ones_f, rhs=M_all[:, j, :], start=True, stop=True),
                 reads=[onesfB, M_allB], writes=[PS[b0 + 1][1]])
            s.op("dve", lambda e, b0=b0: e.tensor_tensor(out=dj, in0=PS[b0][0][:, 0:NE], in1=PBR, op=ALU.add), reads=[PS[b0][1], PBRB], writes=[djB])
            s.op("dve", lambda e, b0=b0: e.tensor_tensor(out=PBR, in0=PS[b0 + 1][0][:, 0:NE], in1=PBR, op=ALU.add), reads=[PS[b0 + 1][1], PBRB], writes=[PBRB])
            for k in range(4):
                s.op("dve", lambda e, j=j, k=k: e.tensor_scalar(out=tq, in0=L_all[:, j, :], scalar1=M4[:, j, k:k + 1], scalar2=None, op0=ALU.is_equal),
                     reads=[L_allB, M4B], writes=[tqB])
                s.op("dve", lambda e: e.tensor_tensor(out=tq, in0=tq, in1=dj, op=ALU.mult), reads=[tqB, djB], writes=[tqB])
                s.op("dve", lambda e, j=j, k=k: e.reduce_sum(out=D4[:, j, k:k + 1], in_=tq, axis=AX.X), reads=[tqB], writes=[D4B])
        s.op("dve", lambda e: e.tensor_scalar(out=D4, in0=D4, scalar1=0.0, scalar2=float(NR - 1), op0=ALU.max, op1=ALU.min), reads=[D4B], writes=[D4B])
        s.op("dve", lambda e: e.tensor_copy(out=D4i, in_=D4), reads=[D4B], writes=[D4iB])
        s.barrier()
        B.reset_arena(KEEP2)
        hp = B.pool("hsc", [D], BF16, 3)
        for j in range(NT):
            hb, hbB = hp[j % 3]
            s.op("sp", lambda e, hb=hb, j=j: e.dma_start(out=hb, in_=hfb.ap()[j * 128:(j + 1) * 128, :]), reads=[db("hfb")], writes=[hbB], dma_buf=hbB)
            for k in range(4):
                s.op("pool", lambda e, hb=hb, j=j, k=k: e.indirect_dma_start(
                    out=xsr.ap()[0:NR, :], out_offset=bass.IndirectOffsetOnAxis(ap=D4i[:, j, k:k + 1], axis=0),
                    in_=hb, in_offset=None),
                    reads=[hbB, D4iB], writes=[db("xsr")], dma_buf=hbB)
        s.barrier()
        B.reset_arena(KEEP2)
        Wg = B.pool("Wg", [8, 2 * FF], BF16, 2)
        Wd = B.pool("Wd", [NFC, D], BF16, 2)
        bgt = B.pool("bgt", [128], F32, 2)
        bcl = B.pool("bcl", [2 * NFC], F32, 2)
        bdb = B.pool("bdb", [D], F32, 2)
        xb = B.pool("xb", [4, D], BF16, 2)
        xT, xTB = B.sb("xT", [8, 512], BF16)
        gg, ggB = B.sb("gg", [512], F32)
        sgm, sgmB = B.sb("sgm", [512], F32)
        uc, ucB = B.sb("uc", [512], F32)
        yT, yTB = B.sb("yT", [NFC, 512], BF16)
        yot, yoB = B.sb("yo", [4, D], F32)
        NG = 2 * NFC
        for b in range(NB):
            wg, wgB = Wg[b % 2]; wd, wdB = Wd[b % 2]; bg, bgB = bgt[b % 2]; bc_, bcB = bcl[b % 2]; bd, bdB = bdb[b % 2]
            s.op("pool", lambda e, wg=wg, b=b: e.indirect_dma_start(out=wg.rearrange("p a b -> p (a b)"), out_offset=None, in_=wgb.ap(),
                                                                  in_offset=bass.IndirectOffsetOnAxis(ap=idxW[:, b:b + 1], axis=0)),
                 reads=[idxWB, db("wgb")], writes=[wgB], dma_buf=wgB)
            s.op("pool", lambda e, wd=wd, b=b: e.indirect_dma_start(out=wd.rearrange("p a b -> p (a b)"), out_offset=None, in_=wdb.ap(),
                                                                  in_offset=bass.IndirectOffsetOnAxis(ap=idxW[:, b:b + 1], axis=0)),
                 reads=[idxWB, db("wdb")], writes=[wdB], dma_buf=wdB)
            s.op("pool", lambda e, bg=bg, b=b: e.indirect_dma_start(out=bg[0:NG, :], out_offset=None, in_=b_gu.ap(),
                                                                  in_offset=bass.IndirectOffsetOnAxis(ap=idx16[0:NG, b:b + 1], axis=0)),
                 reads=[idx16B], writes=[bgB], dma_buf=bgB)
            s.op("pool", lambda e, bd=bd, b=b: e.indirect_dma_start(out=bd, out_offset=None, in_=b_dn.ap(),
                                                                  in_offset=bass.IndirectOffsetOnAxis(ap=idxE[:, b:b + 1], axis=0)),
                 reads=[idxEB], writes=[bdB], dma_buf=bdB)
            xbt, xbB = xb[b % 2]
            s.op("sp", lambda e, xbt=xbt, b=b: e.dma_start(out=xbt, in_=xsr.ap()[b * BS:(b + 1) * BS, :].rearrange("(r p) c -> p r c", p=128)),
                 reads=[db("xsr")], writes=[xbB], dma_buf=xbB)
            s.op("pe", lambda e, bg=bg: e.transpose(out=PS[6][0][:, 0:NG], in_=bg[0:NG, :], identity=id32[0:NG, 0:NG]), reads=[bgB, id32B], writes=[PS[6][1]])
            s.op("act", lambda e, bc_=bc_: e.copy(out=bc_, in_=PS[6][0][:, 0:NG]), reads=[PS[6][1]], writes=[bcB])
            s.op("dve", lambda e, bc_=bc_: e.tensor_scalar(out=bc_[:, NFC:NG], in0=bc_[:, NFC:NG], scalar1=1.0, scalar2=None, op0=ALU.add), reads=[bcB], writes=[bcB])
            for k2 in range(4):
                bank = k2 % 2

                def ftx(e, xbt=xbt, k2=k2, bank=bank):
                    pv = psb(bank)
                    for kk in range(2):
                        for r in range(4):
                            k = 2 * k2 + kk
                            ins = e.transpose(out=pv[:, (kk * 4 + r) * 128:(kk * 4 + r + 1) * 128], in_=xbt[:, r, k * 128:(k + 1) * 128], identity=idb)
                    return ins
                s.op("pe", ftx, reads=[xbB, idbB], writes=[PS[bank][1]])
                s.op("act", lambda e, k2=k2, bank=bank: e.copy(out=xT[:, 2 * k2:2 * k2 + 2, :].rearrange("p a b -> p (a b)"), in_=psb(bank)),
                     reads=[PS[bank][1]], writes=[xTB])
            for i in range(NFC):
                bgk, buk = 2 + 2 * (i % 2), 3 + 2 * (i % 2)

                def fgu(e, wg=wg, i=i, bgk=bgk, buk=buk):
                    for bank, fc in ((bgk, i), (buk, NFC + i)):
                        for k in range(8):
                            ins = e.matmul(PS[bank][0], lhsT=wg[:, k, fc * 128:(fc + 1) * 128], rhs=xT[:, k, :], start=(k == 0), stop=(k == 7))
                    return ins
                s.op("pe", fgu, reads=[wgB, xTB], writes=[PS[bgk][1], PS[buk][1]])
                s.op("dve", lambda e, bgk=bgk, bc_=bc_, i=i: e.tensor_scalar(out=gg, in0=PS[bgk][0], scalar1=bc_[:, i:i + 1], scalar2=7.0, op0=ALU.add, op1=ALU.min),
                     reads=[PS[bgk][1], bcB], writes=[ggB])
                s.op("act", lambda e: e.activation(out=sgm, in_=gg, func=AF.Sigmoid, scale=1.702), reads=[ggB], writes=[sgmB])
                s.op("dve", lambda e: e.tensor_tensor(out=sgm, in0=sgm, in1=gg, op=ALU.mult), reads=[sgmB, ggB], writes=[sgmB])
                s.op("dve", lambda e, buk=buk, bc_=bc_, i=i: e.tensor_scalar(out=uc, in0=PS[buk][0], scalar1=bc_[:, NFC + i:NFC + i + 1], scalar2=-6.0, op0=ALU.add, op1=ALU.max),
                     reads=[PS[buk][1], bcB], writes=[ucB])
                s.op("dve", lambda e, i=i: e.scalar_tensor_tensor(out=yT[:, i, :], in0=uc, scalar=8.0, in1=sgm, op0=ALU.min, op1=ALU.mult),
                     reads=[ucB, sgmB], writes=[yTB])
            for rt in range(4):
                for dc in range(2):
                    bank = 6 + (rt * 2 + dc) % 2

                    def fdn(e, wd=wd, rt=rt, dc=dc, bank=bank):
                        for k in range(NFC):
                            ins = e.matmul(PS[bank][0], lhsT=yT[:, k, rt * 128:(rt + 1) * 128], rhs=wd[:, k, dc * 512:(dc + 1) * 512], start=(k == 0), stop=(k == NFC - 1))
                        return ins
                    s.op("pe", fdn, reads=[wdB, yTB], writes=[PS[bank][1]])
                    s.op("dve", lambda e, bd=bd, rt=rt, dc=dc, bank=bank: e.tensor_tensor(out=yot[:, rt, dc * 512:(dc + 1) * 512], in0=PS[bank][0],
                                                                                         in1=bd[:, dc * 512:(dc + 1) * 512], op=ALU.add),
                         reads=[PS[bank][1], bdB, yoB], writes=[yoB])
            s.op("sp", lambda e, b=b: e.dma_start(out=ysr.ap()[b * BS:(b + 1) * BS, :].rearrange("(r p) c -> p r c", p=128), in_=yot),
                 reads=[yoB], writes=[db("ysr")], dma_buf=yoB)
        s.barrier()
        B.reset_arena(KEEP2)
        gfb = [B.sb("gfb%d" % i, [D], F32) for i in range(2)]
        for g_ in range(2):
            load_bc("sp", gfb[g_][0], gfb[g_][1], modv.ap()[l, g_, 5:6, :])
        yk = B.pool("yk", [D], F32, 4)
        xq = B.pool("xq", [D], F32, 2)
        ac = B.pool("acm", [D], F32, 2)
        for j in range(NT):
            x, xB = xq[j % 2]
            s.op("sp", lambda e, x=x, j=j: e.dma_start(out=x, in_=src.ap()[j * 128:(j + 1) * 128, :]), reads=[db(srcname)], writes=[xB], dma_buf=xB)
            a_, aB = ac[j % 2]
            for k in range(4):
                y_, yB = yk[k]
                s.op("pool", lambda e, y_=y_, j=j, k=k: e.indirect_dma_start(
                    out=y_, out_offset=None, in_=ysr.ap()[0:NR, :], in_offset=bass.IndirectOffsetOnAxis(ap=D4i[:, j, k:k + 1], axis=0)),
                    reads=[db("ysr"), D4iB], writes=[yB], dma_buf=yB)
                if k == 0:
                    s.op("dve", lambda e, y_=y_, a_=a_, j=j: e.tensor_scalar(out=a_, in0=y_, scalar1=G4[:, j, 0:1], scalar2=None, op0=ALU.mult),
                         reads=[yB, G4B], writes=[aB])
                else:
                    s.op("dve", lambda e, y_=y_, a_=a_, j=j, k=k: e.scalar_tensor_tensor(out=a_, in0=y_, scalar=G4[:, j, k:k + 1], in1=a_, op0=ALU.mult, op1=ALU.add),
                         reads=[yB, G4B, aB], writes=[aB])
            g_ = grp_of_tile(j)
            s.op("dve", lambda e, a_=a_, g_=g_: e.tensor_tensor(out=a_, in0=a_, in1=gfb[g_][0], op=ALU.mult), reads=[aB, gfb[g_][1]], writes=[aB])
            s.op("dve", lambda e, a_=a_, x=x: e.tensor_tensor(out=a_, in0=a_, in1=x, op=ALU.add), reads=[aB, xB], writes=[aB])
            s.op("sp", lambda e, a_=a_, j=j: e.dma_start(out=dst.ap()[j * 128:(j + 1) * 128, :], in_=a_), reads=[aB], writes=[db(dstname)], dma_buf=aB)
        s.barrier()

    B.reset_arena(KEEP)
    stg = B.pool("stg", [8, 2 * FF], BF16, 2)
    std = B.pool("std", [NFC, D], BF16, 2)
    for ee in range(2 * NE):
        sg_, sgB_ = stg[ee % 2]
        s.op("pool", lambda e, sg_=sg_, ee=ee: e.dma_start(out=sg_, in_=w_gu.ap()[ee].rearrange("(k p) c -> p k c", p=128)), writes=[sgB_], dma_buf=sgB_)
        s.op("sp", lambda e, sg_=sg_, ee=ee: e.dma_start(out=wgb.ap()[ee * 128:(ee + 1) * 128, :], in_=sg_.rearrange("p a b -> p (a b)")),
             reads=[sgB_], writes=[db("wgb")], dma_buf=sgB_)
        sd_, sdB_ = std[ee % 2]
        s.op("pool", lambda e, sd_=sd_, ee=ee: e.dma_start(out=sd_, in_=w_dn.ap()[ee].rearrange("(k p) c -> p k c", p=128)), writes=[sdB_], dma_buf=sdB_)
        s.op("sp", lambda e, sd_=sd_, ee=ee: e.dma_start(out=wdb.ap()[ee * 128:(ee + 1) * 128, :], in_=sd_.rearrange("p a b -> p (a b)")),
             reads=[sdB_], writes=[db("wdb")], dma_buf=sdB_)
    s.barrier()
    if cfg.stop_after == "PW":
        return finish(B, cfg)

    moe(0, x1, "x1", cfg.ET // 128, lambda j: 0 if j < E[0] // 128 else 1, x2, "x2")
    if cfg.stop_after == "P3":
        return finish(B, cfg)

    def proj1_group(g):
        d = DIL[g]
        B.reset_arena(KEEP)
        Wb, WbB = B.sb("Wb", [8, 3072], BF16)
        load_w_bf16(Wb, WbB, w_qkv_b.ap(), g * 3072, 3072)
        gq1, gq1B = B.sb("gq1", [64], F32); gk1, gk1B = B.sb("gk1", [64], F32)
        load_bc("sp", gq1, gq1B, q_norm_b.ap()[g:g + 1, :]); load_bc("sp", gk1, gk1B, k_norm_b.ap()[g:g + 1, :])
        s.op("dve", lambda e: e.tensor_scalar(out=gq1, in0=gq1, scalar1=0.125, scalar2=None, op0=ALU.mult), reads=[gq1B], writes=[gq1B])
        bcm1 = [B.sb("bcm1_%d" % i, [2, D], F32) for i in range(2)]
        for sg in range(2):
            load_bc("sp", bcm1[sg][0][:, 0, :], bcm1[sg][1], modv.ap()[1, sg, 1:2, :])
            load_bc("sp", bcm1[sg][0][:, 1, :], bcm1[sg][1], modv.ap()[1, sg, 0:1, :])
        xt = B.pool("xt1", [D], F32, 2)
        sq, sqB = B.sb("sq1", [D], F32); ss, ssB = B.sb("ss1", [1], F32); t32, t32B = B.sb("t321", [D], F32)
        hm = B.pool("hm1", [D], BF16, 2)
        hTn = B.pool("hTn", [8, 128], BF16, 2)
        hTr = B.pool("hTr", [8, 128], BF16, 2)
        pf, pfB = B.sb("pf", [3072], F32)
        pn, pnB = B.sb("pn", [2048], F32)
        ssh, sshB = B.sb("ssh1", [32], F32)
        qkb = B.pool("qkb", [2048], BF16, 2)
        vxt = B.pool("vxt1", [16, 65], BF16, 2)
        for i in range(2):
            s.op("dve", lambda e, i=i: e.memset(vxt[i][0], 1.0), writes=[vxt[i][1]])
        qkT = B.pool("qkT", [16, 128], BF16, 2)
        it_ = 0
        for sg in range(2):
            NIg = NI[g][sg]
            base = segoff[sg] + H - 64 * d
            for r in range(d):
                for tl in range(NIg // 128):
                    x, xB = xt[it_ % 2]
                    row0 = base + r + d * 128 * tl
                    s.op("sp", lambda e, x=x, row0=row0, d=d: e.dma_start(out=x, in_=bass.AP(x2, row0 * D, [[d * D, 128], [1, D]])),
                         reads=[db("x2")], writes=[xB], dma_buf=xB)
                    hmb, hmbB = hm[it_ % 2]
                    norm_tile(x, xB, bcm1[sg][0][:, 0, :], bcm1[sg][0][:, 1, :], bcm1[sg][1], sq, sqB, ss, ssB, t32, t32B, hmb, hmbB)
                    hn, hnB = hTn[it_ % 2]; hr, hrB = hTr[it_ % 2]
                    transpose8(hmb, hmbB, idb, idbB, 0, hn, hnB, eng="act")
                    transpose8(hmb, hmbB, antib, antibB, 1, hr, hrB, eng="dve")
                    for cc in range(6):
                        src_T, src_TB = (hn, hnB) if cc < 2 else (hr, hrB)
                        proj(src_T, src_TB, Wb, WbB, cc * 512, 512, 2 + cc)
                    for cc in range(6):
                        s.op("act", lambda e, cc=cc: e.copy(out=pf[:, cc * 512:(cc + 1) * 512], in_=PS[2 + cc][0]), reads=[PS[2 + cc][1], pfB], writes=[pfB])
                    headnorm(pf[:, 0:1024], pfB, 16, gq1, gq1B, sq, sqB, ssh, sshB, pn[:, 0:1024], pnB)
                    headnorm(pf[:, 1024:2048], pfB, 16, gk1, gk1B, sq, sqB, ssh, sshB, pn[:, 1024:2048], pnB)
                    qk, qkB = qkb[it_ % 2]
                    s.op("act", lambda e, qk=qk: e.copy(out=qk, in_=pn), reads=[pnB], writes=[qkB])
                    vt, vtB = vxt[it_ % 2]
                    s.op("dve", lambda e, vt=vt: e.tensor_copy(out=vt[:, :, 0:64], in_=pf[:, 2048:3072].rearrange("p (h c) -> p h c", c=64)),
                         reads=[pfB], writes=[vtB])
                    col0 = r * NIg + tl * 128
                    s.op("sp", lambda e, vt=vt, col0=col0, g=g, sg=sg: e.dma_start(out=vx1[g][sg].ap()[col0:col0 + 128, :, :], in_=vt),
                         reads=[vtB], writes=[db("vx1_%d_%d" % (g, sg))], dma_buf=vtB)
                    qT_, qT_B = qkT[it_ % 2]

                    def ftt(e, qk=qk):
                        for half in range(2):
                            pv = psb(half)
                            for pp in range(8):
                                c0 = half * 1024 + pp * 128
                                ins = e.transpose(out=pv[:, pp * 128:(pp + 1) * 128], in_=qk[:, c0:c0 + 128], identity=idb)
                        return ins
                    s.op("pe", ftt, reads=[qkB, idbB], writes=[PS[0][1], PS[1][1]])
                    s.op("act", lambda e, qT_=qT_: e.copy(out=qT_[:, 0:8, :].rearrange("p a b -> p (a b)"), in_=psb(0)), reads=[PS[0][1]], writes=[qT_B])
                    s.op("dve", lambda e, qT_=qT_: e.tensor_copy(out=qT_[:, 8:16, :].rearrange("p a b -> p (a b)"), in_=psb(1)), reads=[PS[1][1], qT_B], writes=[qT_B])
                    s.op("sp", lambda e, qT_=qT_, col0=col0, g=g, sg=sg: e.dma_start(out=qT1[g][sg].ap()[:, :, col0:col0 + 128], in_=qT_[:, 0:8, :]),
                         reads=[qT_B], writes=[db("qT1_%d_%d" % (g, sg))], dma_buf=qT_B)
                    s.op("sp", lambda e, qT_=qT_, col0=col0, g=g, sg=sg: e.dma_start(out=kT1[g][sg].ap()[:, :, col0:col0 + 128], in_=qT_[:, 8:16, :]),
                         reads=[qT_B], writes=[db("kT1_%d_%d" % (g, sg))], dma_buf=qT_B)
                    it_ += 1
        s.barrier()
    for g_i in range(3):
        proj1_group(g_i)
    if cfg.stop_after == "P4":
        return finish(B, cfg)

    if DEBUG.get("skip_to_p5a"):
        for e_ in ENGS:
            s.ops[e_] = []
        for b_ in s.phase_bufs + [p_[1] for p_ in PS]:
            b_.w = []; b_.r = []; b_.dsem = None
        s.dsem_count = [0] * N_DSEM
        s.dsem_next = 0
        s.dma_since_barrier = {}
    B.reset_arena(KEEP)
    tab, tabB = B.sb("tab", [48], F32)
    s.op("dve", lambda e: e.memset(tab[32:64, :], 1.0), writes=[tabB])
    s.op("sp", lambda e: e.dma_start(out=tab[0:32, :], in_=rel_bias.ap()), writes=[tabB], dma_buf=tabB)
    oh, ohB = B.sb("oh", [6, 256], F32)
    s.op("sp", lambda e: e.dma_start(out=oh[0:33], in_=c_oh.ap().rearrange("g a k m -> k (g a) m")), writes=[ohB], dma_buf=ohB)
    fvt, fvtB = B.sb("fvt", [6, 256], F32)
    for g in range(3):
        for ab in range(2):
            def ffv(e, g=g, ab=ab):
                return e.matmul(PS[0][0][0:16, 0:256], lhsT=tab[0:33, g * 16:(g + 1) * 16], rhs=oh[0:33, g * 2 + ab, :], start=True, stop=True)
            s.op("pe", ffv, reads=[tabB, ohB, onesfB], writes=[PS[0][1]])
            s.op("dve", lambda e, g=g, ab=ab: e.tensor_copy(out=fvt[0:16, g * 2 + ab, :], in_=PS[0][0][0:16, 0:256]), reads=[PS[0][1]], writes=[fvtB])
    s.op("sp", lambda e: e.dma_start(out=fvs.ap().rearrange("g a h m -> h (g a) m"), in_=fvt[0:16]), reads=[fvtB], writes=[db("fvs")], dma_buf=fvtB)
    BM, BMB = B.sb("BM", [3, 2, 16, 128], F32)
    for g in range(3):
        for ab in range(2):
            for hh in range(16):
                s.op("sp", lambda e, g=g, ab=ab, hh=hh: e.dma_start(out=BM[:, g, ab, hh, :],
                                                                 in_=bass.AP(fvs, ((g * 2 + ab) * 16 + hh) * 256, [[1, 128], [1, 128]])),
                     reads=[db("fvs")], writes=[BMB], dma_buf=BMB)
    km, kmB = B.sb("km", [NKM], F32)
    s.op("sp", lambda e: e.dma_start(out=km, in_=kmaskT.ap()), writes=[kmB], dma_buf=kmB)
    if cfg.stop_after == "P5a0":
        s.barrier()
        return finish(B, cfg)
    KTl = B.pool("KTl", [8, 256], BF16, 2)
    QTl = B.pool("QTl", [8, 128], BF16, 2)
    VXl = B.pool("VXl", [2, 16, 65], BF16, 2)
    sc, scB = B.sb("sc", [2, 4, 128], F32)
    pT1 = B.pool("pT1", [2, 4, 128], BF16, 2)
    numt = B.pool("numt", [16, 65], F32, 2)
    kcol = 0
    it_ = 0
    ownoff = (0, T[0])
    for g in range(3):
        d = DIL[g]
        for sg in range(2):
            NIg = NI[g][sg]
            nqt = T[sg] // (128 * d)
            for r in range(d):
                for qt in range(nqt):
                    c0 = r * NIg + qt * 128
                    KTt, KTtB = KTl[it_ % 2]; QTt, QTtB = QTl[it_ % 2]; VXt, VXtB = VXl[it_ % 2]
                    s.op("sp", lambda e, KTt=KTt, c0=c0, g=g, sg=sg: e.dma_start(out=KTt, in_=kT1[g][sg].ap()[:, :, c0:c0 + 256]),
                         reads=[db("kT1_%d_%d" % (g, sg))], writes=[KTtB], dma_buf=KTtB)
                    s.op("sp", lambda e, QTt=QTt, c0=c0, g=g, sg=sg: e.dma_start(out=QTt, in_=qT1[g][sg].ap()[:, :, c0 + 64:c0 + 192]),
                         reads=[db("qT1_%d_%d" % (g, sg))], writes=[QTtB], dma_buf=QTtB)
                    s.op("sp", lambda e, VXt=VXt, c0=c0, g=g, sg=sg: e.dma_start(out=VXt, in_=vx1[g][sg].ap()[c0:c0 + 256, :, :].rearrange("(a k) h c -> k a h c", k=128)),
                         reads=[db("vx1_%d_%d" % (g, sg))], writes=[VXtB], dma_buf=VXtB)
                    nt, ntB = numt[it_ % 2]
                    kc = kcol + r * (NIg // 128) + qt
                    STG = DEBUG.get("p5a_stage", 9)
                    if it_ >= DEBUG.get("p5a_max_it", 10 ** 9):
                        it_ += 1
                        continue
                    for hq in range(4 if STG >= 2 else 0):
                        bs_ = 2 * (hq % 2)

                        def fsc(e, hq=hq, bs_=bs_, KTt=KTt, QTt=QTt):
                            for u in range(2):
                                for ab in range(2):
                                    for hx in range(2):
                                        hh = hq * 4 + 2 * hx + u
                                        pp = hh // 2
                                        cb = (ab * 2 + hx) * 128
                                        ins = e.matmul(PS[bs_ + u][0][:, cb:cb + 128], lhsT=KTt[64 * u:64 * u + 64, pp, ab * 128:(ab + 1) * 128],
                                                       rhs=QTt[64 * u:64 * u + 64, pp, :], start=True, stop=True)
                            return ins
                        s.op("pe", fsc, reads=[KTtB, QTtB], writes=[PS[bs_][1], PS[bs_ + 1][1]])
                        if STG < 3:
                            continue
                        for u in range(2):
                            for ab in range(2):
                                s.op("dve", lambda e, hq=hq, ab=ab, u=u, bs_=bs_, g=g, kc=kc: e.scalar_tensor_tensor(
                                    out=sc[:, ab, u:4:2, :], in0=PS[bs_ + u][0][:, ab * 256:(ab + 1) * 256].rearrange("p (a b) -> p a b", a=2),
                                    scalar=km[:, kc + ab:kc + ab + 1], in1=BM[:, g, ab, hq * 4 + u:hq * 4 + 4:2, :], op0=ALU.add, op1=ALU.add),
                                    reads=[PS[bs_ + u][1], kmB, BMB, scB], writes=[scB])
                        pT, pTB = pT1[hq % 2]
                        if STG < 4:
                            continue
                        s.op("act", lambda e, pT=pT: e.activation(out=pT.rearrange("p a b c -> p (a b c)"), in_=sc.rearrange("p a b c -> p (a b c)"), func=AF.Exp),
                             reads=[scB], writes=[pTB])
                        ob = 4 + (hq % 2)
                        if STG < 5:
                            continue

                        def fpv(e, hq=hq, pT=pT, VXt=VXt, ob=ob):
                            for hi_ in range(4):
                                hh = hq * 4 + hi_
                                for ab in range(2):
                                    ins = e.matmul(PS[ob][0][:, hi_ * 65:(hi_ + 1) * 65], lhsT=pT[:, ab, hi_, :], rhs=VXt[:, ab, hh, :], start=(ab == 0), stop=(ab == 1))
                            return ins
                        s.op("pe", fpv, reads=[pTB, VXtB], writes=[PS[ob][1]])
                        if STG < 6:
                            continue
                        s.op("act", lambda e, nt=nt, hq=hq, ob=ob: e.copy(out=nt[:, hq * 4:hq * 4 + 4, :].rearrange("p a b -> p (a b)"), in_=PS[ob][0][:, 0:260]),
                             reads=[PS[ob][1], ntB], writes=[ntB])
                    row0 = ownoff[sg] + r + d * 128 * qt
                    if STG < 7:
                        it_ += 1
                        continue
                    s.op("sp", lambda e, nt=nt, row0=row0, g=g, d=d: e.dma_start(out=bass.AP(num1[g], row0 * 1040, [[d * 1040, 128], [1, 1040]]),
                                                                              in_=nt.rearrange("p a b -> p (a b)")),
                         reads=[ntB], writes=[db("num1_%d" % g)], dma_buf=ntB)
                    it_ += 1
            kcol += d * (NIg // 128)
    s.barrier()
    if cfg.stop_after == "P5a":
        return finish(B, cfg)

    oT1 = B.dscr("oT1", [64, 16, cfg.TT], BF16)
    B.reset_arena(KEEP)
    nm = B.pool("nm", [3, 16, 65], F32, 2)
    rcd, rcdB = B.sb("rcd", [16], F32)
    ob16 = B.pool("ob16", [D], BF16, 2)
    oTs = B.pool("oTs", [16, 128], BF16, 2)
    for j in range(cfg.TT // 128):
        n_, nB = nm[j % 2]
        for g in range(3):
            s.op("sp", lambda e, n_=n_, g=g, j=j: e.dma_start(out=n_[:, g, :, :], in_=num1[g].ap()[j * 128:(j + 1) * 128, :, :]),
                 reads=[db("num1_%d" % g)], writes=[nB], dma_buf=nB)
        s.op("dve", lambda e, n_=n_: e.tensor_tensor(out=n_[:, 0, :, :], in0=n_[:, 0, :, :], in1=n_[:, 1, :, :], op=ALU.add), reads=[nB], writes=[nB])
        s.op("dve", lambda e, n_=n_: e.tensor_tensor(out=n_[:, 0, :, :], in0=n_[:, 0, :, :], in1=n_[:, 2, :, :], op=ALU.add), reads=[nB], writes=[nB])
        s.op("dve", lambda e, n_=n_: e.reciprocal(out=rcd, in_=n_[:, 0, :, 64]), reads=[nB], writes=[rcdB])
        o_, oB = ob16[j % 2]
        s.op("dve", lambda e, n_=n_, o_=o_: e.tensor_tensor(out=o_.rearrange("p (h c) -> p h c", c=64), in0=n_[:, 0, :, 0:64],
                                                         in1=rcd.unsqueeze(2).to_broadcast([128, 16, 64]), op=ALU.mult), reads=[nB, rcdB], writes=[oB])
        oT_, oT_B = oTs[j % 2]
        bank = j % 2

        def fto(e, o_=o_, bank=bank):
            pv = psb(bank)
            for hh in range(16):
                ins = e.transpose(out=pv[0:64, hh * 64:(hh + 1) * 64].rearrange("p c -> p c"), in_=o_[:, hh * 64:(hh + 1) * 64], identity=idb)
            return ins
        def fto2(e, o_=o_, bank=bank):
            for half in range(2):
                pv = psb(2 * bank + half)
                for hh in range(8):
                    ins = e.transpose(out=pv[0:64, hh * 128:(hh + 1) * 128], in_=o_[:, (half * 8 + hh) * 64:(half * 8 + hh + 1) * 64], identity=idb)
            return ins
        s.op("pe", fto2, reads=[oB, idbB], writes=[PS[2 * bank][1], PS[2 * bank + 1][1]])
        s.op("act", lambda e, oT_=oT_, bank=bank: e.copy(out=oT_[0:64, 0:8, :].rearrange("p a b -> p (a b)"), in_=psb(2 * bank)[0:64, :]), reads=[PS[2 * bank][1]], writes=[oT_B])
        s.op("act", lambda e, oT_=oT_, bank=bank: e.copy(out=oT_[0:64, 8:16, :].rearrange("p a b -> p (a b)"), in_=psb(2 * bank + 1)[0:64, :]), reads=[PS[2 * bank + 1][1], oT_B], writes=[oT_B])
        s.op("sp", lambda e, oT_=oT_, j=j: e.dma_start(out=oT1.ap()[:, :, j * 128:(j + 1) * 128], in_=oT_[0:64]), reads=[oT_B], writes=[db("oT1")], dma_buf=oT_B)
    s.barrier()

    def x2_loader(x, xB, sg, jq):
        row = segoff[sg] + H + jq * 128
        s.op("sp", lambda e, x=x, row=row: e.dma_start(out=x, in_=x2.ap()[row:row + 128, :]), reads=[db("x2")], writes=[xB], dma_buf=xB)

    wo_phase(1, lambda sg, jq: oT1.ap()[:, :, ownoff[sg] + jq * 128:ownoff[sg] + (jq + 1) * 128], [db("oT1")], x2_loader,
             lambda sg: sg, [T[0] // 128, T[1] // 128], x3, "x3", w_o_b)
    if cfg.stop_after == "P5":
        return finish(B, cfg)
    moe(1, x3, "x3", cfg.TT // 128, lambda j: 0 if j < T[0] // 128 else 1, yout, "y")
    return finish(B, cfg)


def finish(B, cfg):
    B.s.op("sp", None, reads=[B.dbuf[n] for n in B.dbuf])
    B.s.emit(B.nc, B.stack)
    B.stack.close()
    return B.nc


def _t5_bucket(rel):
    nb = 16
    max_exact = 8
    ret = (rel > 0).astype(np.int32) * nb
    n = np.abs(rel)
    large = max_exact + (np.log(np.maximum(n, 1) / max_exact) / np.log(1024 / max_exact) * (nb - max_exact)).astype(np.int32)
    large = np.minimum(large, nb - 1)
    return (ret + np.where(n < max_exact, n, large)).astype(np.int32)


def _consts(cfg):
    c = {}
    c["c_ident"] = np.eye(128, dtype=np.float32)
    c["c_anti"] = np.eye(128, dtype=np.float32)[::-1].copy()
    c["c_ustrict"] = np.triu(np.ones((128, 128), np.float32), 1)
    NE = cfg.NE
    c["c_une"] = np.stack([np.triu(np.ones((NE, NE), np.float32), 0), np.triu(np.ones((NE, NE), np.float32), 1)])
    NB = cfg.nblk(cfg.ET)
    c["c_iotab"] = np.tile((np.arange(NB, dtype=np.float32) * BS)[None, :], (NE, 1))
    c["c_piota"] = np.arange(128, dtype=np.float32).reshape(128, 1)
    SP0 = cfg.S[0] + 2 * H
    t = np.arange(SP0, dtype=np.float64) - H
    row = np.floor(t / 64.0).astype(np.float32)
    col = (t - np.floor(t / 64.0) * 64).astype(np.float32)
    inv = (1.0 / (np.float32(10000.0) ** (np.arange(16, dtype=np.float32) / np.float32(16)))).astype(np.float32)
    ar = row[:, None] * inv[None, :]
    ac = col[:, None] * inv[None, :]
    ang = np.concatenate([ar, ar, ac, ac], axis=-1).astype(np.float32)
    sgn = np.concatenate([-np.ones(16), np.ones(16), -np.ones(16), np.ones(16)]).astype(np.float32)
    c["ropep"] = np.concatenate([np.cos(ang), np.sin(ang) * sgn[None, :]], axis=-1).astype(np.float32)
    oh = np.zeros((3, 2, 33, 256), np.float32)
    for g, d in enumerate(DIL):
        for ab, off in enumerate((63, 191)):
            m = np.arange(256)
            delta = off - m
            ok = np.abs(delta) <= 64
            bk = _t5_bucket(delta * d)
            for mm in range(256):
                if ok[mm]:
                    oh[g, ab, bk[mm], mm] = 1.0
                else:
                    oh[g, ab, 32, mm] = NEG
    c["c_oh"] = oh
    return c


def _kmask(cfg, q):
    cols = []
    for g, d in enumerate(DIL):
        for sg in range(2):
            Tn, Sn = cfg.T[sg], cfg.S[sg]
            NI = Tn // d + 128
            L = Sn // d
            for r in range(d):
                for tl in range(NI // 128):
                    a = np.arange(128)
                    ik = tl * 128 + (127 - a)
                    gi = (q * Tn) // d + ik - 64
                    cols.append(np.where((gi >= 0) & (gi < L), 0.0, NEG).astype(np.float32))
    return np.stack(cols, axis=1)


def make_in_maps(cfg, inp):
    consts = _consts(cfg)
    maps = []
    NE, FF = cfg.NE, cfg.FF
    shared = {
        "norm_mix_g": inp["norm_mix_g"], "norm_ffn_g": inp["norm_ffn_g"], "w_ada": inp["w_ada"], "b_ada": inp["b_ada"],
        "w_qkv_a": inp["w_qkv_a"][0], "q_norm_a": inp["q_norm_a"], "k_norm_a": inp["k_norm_a"], "w_o_a": inp["w_o_a"][0],
        "w_qkv_b": inp["w_qkv_b"][0], "q_norm_b": inp["q_norm_b"][0], "k_norm_b": inp["k_norm_b"][0], "w_o_b": inp["w_o_b"][0],
        "rel_bias": inp["rel_bias"], "w_router": inp["w_router"], "b_router": inp["b_router"],
        "w_gate_up": inp["w_gate_up"].reshape(2 * NE, D, 2 * FF), "b_gate_up": inp["b_gate_up"].reshape(2 * NE * (2 * FF // 128), 128),
        "w_down": inp["w_down"].reshape(2 * NE, FF, D), "b_down": inp["b_down"].reshape(2 * NE, D),
    }
    shared = {k: np.ascontiguousarray(v, dtype=np.float32) for k, v in shared.items()}
    ropep = consts.pop("ropep")
    for c in range(8):
        b, q = c // 4, c % 4
        m = dict(shared)
        m.update(consts)
        m["xs0"] = np.ascontiguousarray(inp["x_sample"][b], dtype=np.float32)
        m["xs1"] = np.ascontiguousarray(inp["x_prompt"][b], dtype=np.float32)
        xwl, rwl = [], []
        for sg, key in enumerate(("x_sample", "x_prompt")):
            Tn, Sn, En = cfg.T[sg], cfg.S[sg], cfg.E[sg]
            w = np.zeros((En, D), np.float32)
            lo = q * Tn - H
            a, bnd = max(lo, 0), min(lo + En, Sn)
            w[a - lo:bnd - lo] = inp[key][b][a:bnd]
            xwl.append(w)
            rwl.append(ropep[q * Tn:q * Tn + En])
        m["xw"] = np.concatenate(xwl, axis=0)
        m["ropew"] = np.ascontiguousarray(np.concatenate(rwl, axis=0))
        m["ropek"] = np.ascontiguousarray(ropep[H:H + cfg.S[0]])
        m["cvec"] = np.stack([inp["c_sample"][b], inp["c_prompt"][b]]).astype(np.float32)
        m["kmaskT"] = _kmask(cfg, q)
        maps.append(m)
    return maps


def run(cfg, inp):
    nc = build(cfg)
    maps = make_in_maps(cfg, inp)
    res = run_bass_kernel_spmd(nc, maps, core_ids=list(range(8)))
    return res.results


def kernel(**inputs):
    cfg = Cfg()
    res = run(cfg, inputs)
    ys = np.zeros((2, cfg.S[0], D), np.float32)
    yp = np.zeros((2, cfg.S[1], D), np.float32)
    for c in range(8):
        b, q = c // 4, c % 4
        y = np.asarray(res[c]["y"])
        ys[b, q * cfg.T[0]:(q + 1) * cfg.T[0]] = y[:cfg.T[0]]
        yp[b, q * cfg.T[1]:(q + 1) * cfg.T[1]] = y[cfg.T[0]:]
    return (yp, ys)
```
